# Optimizing a Trainium2 kernel written in Bass

```python
import numpy as np
import jax
import jax.numpy as jnp
from jax import lax

D_MODEL = 1024
BATCH = 8
SEQ = 2048
DEPTH = 1

CHUNK = 64
POOL_WIDTH = D_MODEL
POOL_WINDOWS = (2, 4, 8, 16)
POOL_GROUPS = len(POOL_WINDOWS)
POOL_GROUP_WIDTH = POOL_WIDTH // POOL_GROUPS
GLA_HEADS = 4
GLA_KEY_DIM = D_MODEL // 2
GLA_VALUE_DIM = D_MODEL
GLA_HEAD_K = GLA_KEY_DIM // GLA_HEADS
GLA_HEAD_V = GLA_VALUE_DIM // GLA_HEADS
GLA_GATE_RANK = 16
GLA_TAU = 16.0
N_EXPERTS = 32
TOP_K = 4
D_EXPERT = D_MODEL
SWIGLU_ALPHA = 1.702
SWIGLU_LIMIT = 7.0
EXPERT_BLOCK = 256
LN_EPS = 1e-5
RMS_EPS = 1e-6
DEEPNORM_ALPHA = (2.0 * DEPTH) ** 0.25
DEEPNORM_BETA = (8.0 * DEPTH) ** -0.25
IN_WIDTHS = (POOL_WIDTH, GLA_KEY_DIM, GLA_KEY_DIM, GLA_VALUE_DIM, GLA_VALUE_DIM, GLA_GATE_RANK, D_MODEL, D_MODEL)
IN_TOTAL = sum(IN_WIDTHS)
IN_OFFSETS = tuple(int(o) for o in np.cumsum(IN_WIDTHS)[:-1])
V_START = POOL_WIDTH + 2 * GLA_KEY_DIM
V_END = V_START + GLA_VALUE_DIM

kernel_name = 'hybrid_pool_gla_moe_deepnorm_block'


def layer_norm(x, gain, bias):
    x32 = x.astype(jnp.float32)
    mu = jnp.mean(x32, axis=-1, keepdims=True)
    var = jnp.mean(jnp.square(x32 - mu), axis=-1, keepdims=True)
    return ((x32 - mu) * lax.rsqrt(var + LN_EPS) * gain + bias).astype(x.dtype)


def multiscale_pool(a):
    bsz, seq, _ = a.shape
    a32 = a.astype(jnp.float32)
    csum = jnp.concatenate([jnp.zeros((bsz, 1, POOL_WIDTH), jnp.float32), jnp.cumsum(a32, axis=1)], axis=1)
    t = jnp.arange(seq)
    groups = []
    for g, w in enumerate(POOL_WINDOWS):
        lo_c, hi_c = g * POOL_GROUP_WIDTH, (g + 1) * POOL_GROUP_WIDTH
        cs = csum[:, :, lo_c:hi_c]
        start = jnp.maximum(t + 1 - w, 0)
        win_sum = cs[:, 1:, :] - cs[:, start, :]
        count = jnp.minimum(t + 1, w).astype(jnp.float32)
        groups.append(win_sum / count[None, :, None] - a32[:, :, lo_c:hi_c])
    return jnp.stack(groups, axis=2)


def gla_chunked(q, k, v, log_a):
    bsz, seq, heads, dk = q.shape
    dv = v.shape[-1]
    n_chunks = seq // CHUNK

    def to_chunks(t):
        return t.astype(jnp.float32).reshape(bsz, n_chunks, CHUNK, heads, -1).transpose(1, 0, 3, 2, 4)

    qc = to_chunks(q) * (dk ** -0.5)
    kc, vc, gc = to_chunks(k), to_chunks(v), to_chunks(log_a)
    causal = jnp.tril(jnp.ones((CHUNK, CHUNK), dtype=bool))[:, :, None]

    def step(state, inp):
        qi, ki, vi, gi = inp
        b = jnp.cumsum(gi, axis=-2)
        diff = b[:, :, :, None, :] - b[:, :, None, :, :]
        decay = jnp.where(causal, jnp.exp(jnp.where(causal, diff, 0.0)), 0.0)
        scores = jnp.einsum('bhid,bhjd,bhijd->bhij', qi, ki, decay)
        o = jnp.einsum('bhij,bhje->bhie', scores, vi) + jnp.einsum('bhid,bhde->bhie', qi * jnp.exp(b), state)
        b_last = b[:, :, -1:, :]
        state = jnp.exp(b_last[:, :, 0, :])[..., None] * state + jnp.einsum('bhjd,bhje->bhde', ki * jnp.exp(b_last - b), vi)
        return state, o

    state0 = jnp.zeros((bsz, heads, dk, dv), jnp.float32)
    _, out = lax.scan(step, state0, (qc, kc, vc, gc))
    return out.transpose(1, 0, 3, 2, 4).reshape(bsz, seq, heads, dv)


def token_mixer(u, w_in, w_pool_group, pool_scale, w_branch_a, w_alpha_up, b_alpha, gla_norm_gain, w_branch_b, w_out):
    bsz, seq, _ = u.shape
    proj = u @ w_in
    a, q, k, v, r, alpha_low, gate_a, gate_b = jnp.split(proj, IN_OFFSETS, axis=-1)
    pooled = multiscale_pool(a).astype(u.dtype)
    ya = jnp.einsum('bsgc,gcd->bsgd', pooled, w_pool_group).reshape(bsz, seq, POOL_WIDTH) * pool_scale
    ya = ya @ w_branch_a
    log_a = jax.nn.log_sigmoid((alpha_low @ w_alpha_up + b_alpha).astype(jnp.float32)) / GLA_TAU
    o = gla_chunked(q.reshape(bsz, seq, GLA_HEADS, GLA_HEAD_K), k.reshape(bsz, seq, GLA_HEADS, GLA_HEAD_K),
                    v.reshape(bsz, seq, GLA_HEADS, GLA_HEAD_V), log_a.reshape(bsz, seq, GLA_HEADS, GLA_HEAD_K))
    o = o * lax.rsqrt(jnp.mean(jnp.square(o), axis=-1, keepdims=True) + RMS_EPS) * gla_norm_gain
    yb = (o.reshape(bsz, seq, GLA_VALUE_DIM).astype(u.dtype) * jax.nn.silu(r)) @ w_branch_b
    merged = jax.nn.sigmoid(gate_a) * ya + jax.nn.sigmoid(gate_b) * yb
    return merged @ w_out


def routed_moe(u, w_router, b_router, w_gate_up, b_gate_up, w_down, b_down):
    bsz, seq, d = u.shape
    n_tok = bsz * seq
    xt = u.reshape(n_tok, d)
    logits = (xt @ w_router + b_router).astype(jnp.float32)
    top_val, top_idx = lax.top_k(logits, TOP_K)
    weights = jax.nn.softmax(top_val, axis=-1)
    n_assign = n_tok * TOP_K
    flat_e = top_idx.reshape(-1).astype(jnp.int32)
    order = jnp.argsort(flat_e)
    sorted_e = flat_e[order]
    counts = jnp.bincount(flat_e, length=N_EXPERTS)
    padded = (counts + EXPERT_BLOCK - 1) // EXPERT_BLOCK * EXPERT_BLOCK
    pad_end = jnp.cumsum(padded)
    pad_start = pad_end - padded
    grp_start = jnp.cumsum(counts) - counts
    dest = pad_start[sorted_e] + (jnp.arange(n_assign) - grp_start[sorted_e])
    n_rows = (n_assign + N_EXPERTS * (EXPERT_BLOCK - 1) + EXPERT_BLOCK - 1) // EXPERT_BLOCK * EXPERT_BLOCK
    n_blocks = n_rows // EXPERT_BLOCK
    row_token = jnp.full((n_rows,), n_tok, jnp.int32).at[dest].set((order // TOP_K).astype(jnp.int32))
    block_expert = jnp.minimum(jnp.searchsorted(pad_end, jnp.arange(n_blocks) * EXPERT_BLOCK, side='right'), N_EXPERTS - 1)
    x_pad = jnp.concatenate([xt, jnp.zeros((1, d), xt.dtype)], axis=0)
    x_rows = x_pad[row_token].reshape(n_blocks, EXPERT_BLOCK, d)

    def expert_block(args):
        xb, e = args
        h = xb @ w_gate_up[e] + b_gate_up[e]
        gate = jnp.minimum(h[:, ::2], SWIGLU_LIMIT)
        up = jnp.clip(h[:, 1::2], -SWIGLU_LIMIT, SWIGLU_LIMIT)
        glu = gate * jax.nn.sigmoid(gate * SWIGLU_ALPHA)
        return ((up + 1.0) * glu) @ w_down[e] + b_down[e]

    y_rows = lax.map(expert_block, (x_rows, block_expert)).reshape(n_rows, d)
    row_of_assign = jnp.zeros((n_assign,), jnp.int32).at[order].set(dest.astype(jnp.int32))
    y_sel = y_rows[row_of_assign].reshape(n_tok, TOP_K, d)
    y = jnp.einsum('tk,tkd->td', weights.astype(y_sel.dtype), y_sel)
    return y.reshape(bsz, seq, d)


def setup_inputs(seed: int = 0) -> dict:
    key = jax.random.key(seed)
    ks = jax.random.split(key, 24)

    def nrm(k, shape, scale):
        return jax.random.normal(k, shape, jnp.float32) * scale

    v_col_scale = jnp.ones((IN_TOTAL,), jnp.float32).at[V_START:V_END].set(DEEPNORM_BETA)
    return {
        'x': nrm(ks[0], (BATCH, SEQ, D_MODEL), 1.0),
        'c': nrm(ks[1], (BATCH, D_MODEL), 1.0),
        'w_ada': nrm(ks[2], (DEPTH, D_MODEL, 6 * D_MODEL), 0.5 * D_MODEL ** -0.5),
        'b_ada': nrm(ks[3], (DEPTH, 6 * D_MODEL), 0.02),
        'w_in': nrm(ks[4], (DEPTH, D_MODEL, IN_TOTAL), D_MODEL ** -0.5) * v_col_scale,
        'w_pool_group': nrm(ks[5], (DEPTH, POOL_GROUPS, POOL_GROUP_WIDTH, POOL_GROUP_WIDTH), POOL_GROUP_WIDTH ** -0.5),
        'pool_scale': 1.0 + nrm(ks[6], (DEPTH, POOL_WIDTH), 0.1),
        'w_branch_a': nrm(ks[7], (DEPTH, POOL_WIDTH, D_MODEL), POOL_WIDTH ** -0.5 * DEEPNORM_BETA),
        'w_alpha_up': nrm(ks[8], (DEPTH, GLA_GATE_RANK, GLA_KEY_DIM), GLA_GATE_RANK ** -0.5),
        'b_alpha': nrm(ks[9], (DEPTH, GLA_KEY_DIM), 0.1),
        'gla_norm_gain': 1.0 + nrm(ks[10], (DEPTH, GLA_HEAD_V), 0.1),
        'w_branch_b': nrm(ks[11], (DEPTH, GLA_VALUE_DIM, D_MODEL), GLA_VALUE_DIM ** -0.5 * DEEPNORM_BETA),
        'w_out': nrm(ks[12], (DEPTH, D_MODEL, D_MODEL), D_MODEL ** -0.5 * DEEPNORM_BETA),
        'ln1_gain': 1.0 + nrm(ks[13], (DEPTH, D_MODEL), 0.05),
        'ln1_bias': nrm(ks[14], (DEPTH, D_MODEL), 0.02),
        'w_router': nrm(ks[15], (DEPTH, D_MODEL, N_EXPERTS), D_MODEL ** -0.5),
        'b_router': nrm(ks[16], (DEPTH, N_EXPERTS), 0.01),
        'w_gate_up': nrm(ks[17], (DEPTH, N_EXPERTS, D_MODEL, 2 * D_EXPERT), D_MODEL ** -0.5 * DEEPNORM_BETA),
        'b_gate_up': nrm(ks[18], (DEPTH, N_EXPERTS, 2 * D_EXPERT), 0.02),
        'w_down': nrm(ks[19], (DEPTH, N_EXPERTS, D_EXPERT, D_MODEL), D_EXPERT ** -0.5 * DEEPNORM_BETA),
        'b_down': nrm(ks[20], (DEPTH, N_EXPERTS, D_MODEL), 0.02),
        'ln2_gain': 1.0 + nrm(ks[21], (DEPTH, D_MODEL), 0.05),
        'ln2_bias': nrm(ks[22], (DEPTH, D_MODEL), 0.02),
    }


def reference(x, c, w_ada, b_ada, w_in, w_pool_group, pool_scale, w_branch_a, w_alpha_up, b_alpha,
              gla_norm_gain, w_branch_b, w_out, ln1_gain, ln1_bias, w_router, b_router,
              w_gate_up, b_gate_up, w_down, b_down, ln2_gain, ln2_bias):
    for l in range(DEPTH):
        mod = jax.nn.silu(c) @ w_ada[l] + b_ada[l]
        sh_m, sc_m, g_m, sh_f, sc_f, g_f = jnp.split(mod[:, None, :], 6, axis=-1)
        u = x * (1.0 + sc_m) + sh_m
        y = token_mixer(u, w_in[l], w_pool_group[l], pool_scale[l], w_branch_a[l], w_alpha_up[l], b_alpha[l],
                        gla_norm_gain[l], w_branch_b[l], w_out[l])
        x = layer_norm(DEEPNORM_ALPHA * x + g_m * y, ln1_gain[l], ln1_bias[l])
        u = x * (1.0 + sc_f) + sh_f
        y = routed_moe(u, w_router[l], b_router[l], w_gate_up[l], b_gate_up[l], w_down[l], b_down[l])
        x = layer_norm(DEEPNORM_ALPHA * x + g_f * y, ln2_gain[l], ln2_bias[l])
    return x
```

```python
import numpy as np
from contextlib import ExitStack
import concourse.bass as bass
import concourse.mybir as mybir
from concourse.bass_utils import run_bass_kernel_spmd

F32 = mybir.dt.float32
BF16 = mybir.dt.bfloat16
AF = mybir.ActivationFunctionType
ALU = mybir.AluOpType
AX = mybir.AxisListType

ENGS = ["tensor", "vector", "scalar", "gpsimd", "sync"]
T = 2048
NT = 16
D = 1024
KC = 8
NE = 32
IN_TOTAL = 6160
BLK = 512
CAP = BLK
NBLK = (T * 4 + NE * (BLK - 1) + BLK - 1) // BLK
NROWS = NBLK * BLK
I32 = mybir.dt.int32
U32 = mybir.dt.uint32
ALPHA = 2.0 ** 0.25
C7 = 1.702 * 7.0 / (1.0 + float(np.exp(-1.702 * 7.0)))


class Tok:
    __slots__ = ("w", "r", "sem")

    def __init__(self):
        self.w = {}
        self.r = {}
        self.sem = None


class Tile(Tok):
    __slots__ = ("t",)

    def __init__(self, t):
        Tok.__init__(self)
        self.t = t

    def __getitem__(self, k):
        return self.t[k]


class Sched:
    def __init__(self, nc, stack):
        self.nc = nc
        self.stack = stack
        self.q = {e: [] for e in ENGS}
        self.sems = {}
        self.count = {}
        self.seen = {e: {} for e in ENGS}
        for e in ENGS:
            self.sems[e] = stack.enter_context(nc.semaphore("s_" + e))
            self.count[e] = 0
        self.ndma = 0
        self.dead = False
        self.free_sems = []
        self.sem_order = []
        self.max_dma_sems = 100
        self.rr = 0
        self.last_mark = 0

    def mark(self):
        return len(self.sem_order)

    def recycle(self, mark):
        for sm in self.sem_order[mark:]:
            if sm not in self.free_sems:
                self.free_sems.append(sm)
        del self.sem_order[mark:]

    def _waits(self, eng, need):
        out = []
        for k, v in need.items():
            if v > self.seen[eng].get(k, 0):
                self.seen[eng][k] = v
                out.append((k, v))
        return out

    @staticmethod
    def _merge(need, d):
        for k, v in d.items():
            if v > need.get(k, 0):
                need[k] = v

    def _deps(self, reads, writes):
        need = {}
        for t in reads:
            self._merge(need, t.w)
        for t in writes:
            self._merge(need, t.w)
            self._merge(need, t.r)
        return need

    def _commit(self, tk, reads, writes):
        k, v = tk
        for t in reads:
            if v > t.r.get(k, 0):
                t.r[k] = v
        for t in writes:
            t.w = {k: v}
            t.r = {}

    def do(self, eng, fn, reads=(), writes=()):
        if self.dead:
            return (eng, 0)
        need = self._deps(reads, writes)
        w = self._waits(eng, need)
        self.count[eng] += 1
        tk = (eng, self.count[eng])
        self.q[eng].append((w, fn, (eng, 1)))
        self._commit(tk, reads, writes)
        return tk

    def dma(self, eng, fn, reads=(), writes=()):
        if self.dead:
            return (eng, 0)
        tok = writes[0]
        if tok.sem is None:
            fl = [x for x in self.free_sems if x.startswith(eng[0])]
            if fl:
                tok.sem = fl[0]
                self.free_sems.remove(fl[0])
            else:
                self.ndma += 1
                tok.sem = "%s%d" % (eng[0], self.ndma)
                self.sems[tok.sem] = self.stack.enter_context(self.nc.semaphore("s_" + tok.sem))
                self.count[tok.sem] = 0
            self.sem_order.append(tok.sem)
        sem = tok.sem
        need = self._deps(reads, writes)
        if self.count[sem] > need.get(sem, 0):
            need[sem] = self.count[sem]
        w = self._waits(eng, need)
        fns = fn if isinstance(fn, (list, tuple)) else [fn]
        for j, f1 in enumerate(fns):
            self.count[sem] += 16
            self.q[eng].append((w if j == 0 else [], f1, (sem, 16)))
        tk = (sem, self.count[sem])
        self._commit(tk, reads, writes)
        return tk

    def raw(self, eng, fn, reads=()):
        if self.dead:
            return
        need = self._deps(reads, ())
        self.q[eng].append((self._waits(eng, need), fn, None))

    def barrier(self, force=False):
        if self.dead and not force:
            return
        need = dict(self.count)
        for e in ENGS:
            self.q[e].append((self._waits(e, dict(need)), None, None))
        self.recycle(self.last_mark)
        self.last_mark = self.mark()

    def emit(self, block):
        for e_name in ENGS:
            q = self.q[e_name]

            def body(engine, q=q):
                for (w, fn, inc) in q:
                    for (k, v) in w:
                        engine.wait_ge(self.sems[k], v)
                    if fn is None:
                        continue
                    ins = fn(engine)
                    if inc is not None:
                        ins.then_inc(self.sems[inc[0]], inc[1])

            getattr(block, e_name)(body)


class _Stop(Exception):
    pass


class Ring:
    def __init__(self, tiles):
        self.tiles = tiles
        self.i = 0

    def get(self):
        t = self.tiles[self.i % len(self.tiles)]
        self.i += 1
        return t


def build(dbg=False, stop=None):
    nc = bass.Bass("TRN2", target_bir_lowering=False)

    def din(name, shape):
        return nc.dram_tensor(name, shape, F32, kind="ExternalInput").ap()

    x = din("x", [T, D])
    cT = din("cT", [128, 8])
    w_ada = din("w_ada", [D, 6 * D])
    b_ada = din("b_ada", [1, 6 * D])
    w_in = din("w_in", [D, IN_TOTAL])
    wpg = din("wpg", [4, 256, 256])
    pscT = din("pscT", [128, 8])
    wba = din("wba", [D, D])
    wau = din("wau", [16, 512])
    baT = din("baT", [128, 4])
    gainT = din("gainT", [128, 2])
    wbb = din("wbb", [D, D])
    wout = din("wout", [D, D])
    ln1g = din("ln1g", [1, D])
    ln1b = din("ln1b", [1, D])
    wr = din("wr", [D, NE])
    br = din("br", [1, NE])
    w1g_h = nc.dram_tensor("w1g", [NE, D, D], F32, kind="ExternalInput")
    w1u_h = nc.dram_tensor("w1u", [NE, D, D], F32, kind="ExternalInput")
    b1gE_h = nc.dram_tensor("b1gE", [NE, 128, 8], F32, kind="ExternalInput")
    b1uE_h = nc.dram_tensor("b1uE", [NE, 128, 8], F32, kind="ExternalInput")
    w2_h = nc.dram_tensor("w2", [NE, D, D], F32, kind="ExternalInput")
    b2_h = nc.dram_tensor("b2", [NE, D], F32, kind="ExternalInput")
    w1g2 = w1g_h.ap().rearrange("e r n -> (e r) n")
    w1u2 = w1u_h.ap().rearrange("e r n -> (e r) n")
    w22 = w2_h.ap().rearrange("e r n -> (e r) n")
    b1gE2 = b1gE_h.ap().rearrange("e p f -> (e p) f")
    b1uE2 = b1uE_h.ap().rearrange("e p f -> (e p) f")
    b2v = b2_h.ap()
    ln2g = din("ln2g", [1, D])
    ln2b = din("ln2b", [1, D])
    out = nc.dram_tensor("out", [T, D], F32, kind="ExternalOutput").ap()
    x1s = nc.dram_tensor("x1s", [T, D], F32).ap()
    xdisp = nc.dram_tensor("xdisp", [NROWS, D], BF16).ap()
    ydisp = nc.dram_tensor("ydisp", [NROWS, D], F32).ap()
    u2s = nc.dram_tensor("u2s", [T, D], BF16).ap()
    dbg_out = {}
    if dbg:
        for nm, shp in [("d_mod", [128, 6 * D]), ("d_u1T", [128, 8 * T]), ("d_obT", [128, 8 * T]), ("d_ya1T", [128, 8 * T]),
                        ("d_mT", [128, 8 * T]), ("d_x1", [T, D])]:
            dbg_out[nm] = nc.dram_tensor(nm, shp, F32, kind="ExternalOutput").ap()

    w_in_v = w_in.rearrange("(k p) n -> p k n", p=128)
    w_ada_v = w_ada.rearrange("(k p) n -> p k n", p=128)
    wba_v = wba.rearrange("(k p) n -> p k n", p=128)
    wbb_v = wbb.rearrange("(k p) n -> p k n", p=128)
    wout_v = wout.rearrange("(k p) n -> p k n", p=128)
    wr_v = wr.rearrange("(k p) n -> p k n", p=128)

    with ExitStack() as st0:
        S = Sched(nc, st0)
        bnd_reg = st0.enter_context(nc.gpsimd.register("bnd_reg"))
        bw_reg = st0.enter_context(nc.gpsimd.register("bw_reg"))
        bb_reg = st0.enter_context(nc.gpsimd.register("bb_reg"))
        b2_reg = st0.enter_context(nc.gpsimd.register("b2_reg"))
        S.q["gpsimd"].append(([], lambda e: e.reg_mov(bw_reg, NE * D - 1), None))
        S.q["gpsimd"].append(([], lambda e: e.reg_mov(bb_reg, NE * 128 - 1), None))
        S.q["gpsimd"].append(([], lambda e: e.reg_mov(b2_reg, NE - 1), None))
        S.q["gpsimd"].append(([], lambda e: e.reg_mov(bnd_reg, NROWS - 1), None))
        try:

            uniq = [0]

            def sb(stack, name, shape, dt):
                uniq[0] += 1
                return Tile(stack.enter_context(nc.sbuf_tensor("%s_%d" % (name, uniq[0]), shape, dt)))

            def ps(stack, name, shape, dt):
                uniq[0] += 1
                return Tile(stack.enter_context(nc.psum_tensor("%s_%d" % (name, uniq[0]), shape, dt)))

            def mm(outt, out_ap, pairs, reads):
                def fn(e):
                    ins = None
                    n = len(pairs)
                    for i, (l, r) in enumerate(pairs):
                        ins = e.matmul(out_ap, lhsT=l, rhs=r, start=(i == 0), stop=(i == n - 1))
                    return ins
                return S.do("tensor", fn, reads=reads, writes=[outt])

            def dbg_dump(name, tile, ap, tmpdt=None):
                if not dbg:
                    return
                S.barrier()
                if tmpdt is None:
                    S.dma("sync", lambda e: e.dma_start(out=dbg_out[name], in_=ap), reads=[tile], writes=[Tok()])
                else:
                    S.dma("gpsimd", lambda e: e.dma_start(out=dbg_out[name], in_=ap), reads=[tile], writes=[Tok()])
                S.barrier()

            ident_bf = sb(st0, "ident_bf", [128, 128], BF16)
            ident_f = sb(st0, "ident_f", [128, 128], F32)
            ones_bf = sb(st0, "ones_bf", [128, 128], BF16)
            cmask4 = sb(st0, "cmask4", [128, 512], F32)
            mask01 = sb(st0, "mask01", [128, 512], F32)
            rc16 = sb(st0, "rc16", [128, 16], F32)
            gf_bc = sb(st0, "gf_bc", [128, D], F32)
            uT_tok = [Tok() for _ in range(NT)]
            gidx_all = sb(st0, "gidx_all", [128, NT * 4], I32)
            w4_all = sb(st0, "w4_all", [128, NT * 4], F32)
            e4_all = sb(st0, "e4_all", [128, NT * 4], F32)
            p4_all = sb(st0, "p4_all", [128, NT * 4], F32)
            e4a_tok, p4a_tok = Tok(), Tok()
            idxW = sb(st0, "idxW", [128, NBLK * 8], I32)
            idxB = sb(st0, "idxB", [128, NBLK], I32)
            idxB2 = sb(st0, "idxB2", [128, NBLK], I32)
            idx_tok = Tok()
            u2s_tok = Ring([Tok() for _ in range(4)])
            iota32 = sb(st0, "iota32", [128, NE], F32)
            Ltri = sb(st0, "Ltri", [128, 128], BF16)
            xz_tok = Ring([Tok() for _ in range(4)])
            sc_tok = Ring([Tok() for _ in range(8)])
            yw_tok = Ring([Tok() for _ in range(8)])

            def uT_reads(tt):
                return uT_tok[tt * 4:(tt + 1) * 4]

            S.do("gpsimd", lambda e: e.memset(ones_bf[:], 1.0), writes=[ones_bf])
            S.do("gpsimd", lambda e: e.affine_select(out=ident_bf[:], in_=ones_bf[:], pattern=[[-1, 128]],
                                                     compare_op=ALU.is_equal, fill=0.0, base=0, channel_multiplier=1),
                 reads=[ones_bf], writes=[ident_bf])
            S.do("vector", lambda e: e.tensor_copy(out=ident_f[:], in_=ident_bf[:]), reads=[ident_bf], writes=[ident_f])
            S.do("gpsimd", lambda e: e.memset(cmask4[:], 1.0), writes=[cmask4])
            S.do("gpsimd", lambda e: e.affine_select(out=cmask4[:], in_=cmask4[:], pattern=[[0, 4], [1, 128]],
                                                     compare_op=ALU.is_ge, fill=0.0, base=0, channel_multiplier=-1),
                 reads=[cmask4], writes=[cmask4])
            S.do("gpsimd", lambda e: e.memset(mask01[:], 1.0), writes=[mask01])
            S.do("gpsimd", lambda e: e.memset(mask01[:].rearrange("p (c j) -> p c j", j=128)[:, :, 0:1], 0.0),
                 writes=[mask01])
            S.do("gpsimd", lambda e: e.iota(rc16[:], pattern=[[1, 16]], base=1, channel_multiplier=0,
                                            allow_small_or_imprecise_dtypes=True), writes=[rc16])
            S.do("vector", lambda e: e.reciprocal(out=rc16[:], in_=rc16[:]), reads=[rc16], writes=[rc16])
            S.do("gpsimd", lambda e: e.iota(iota32[:], pattern=[[1, NE]], base=0, channel_multiplier=0,
                                            allow_small_or_imprecise_dtypes=True), writes=[iota32])
            S.do("gpsimd", lambda e: e.memset(Ltri[:], 1.0), writes=[Ltri])
            S.do("gpsimd", lambda e: e.affine_select(out=Ltri[:], in_=Ltri[:], pattern=[[1, 128]], compare_op=ALU.is_gt,
                                                     fill=0.0, base=0, channel_multiplier=-1), writes=[Ltri])

            with ExitStack() as stM:
                uT = sb(stM, "uT", [128, 8, T], BF16)
                mod_bc = sb(stM, "mod_bc", [128, 6 * D], F32)
                SH_M, SC_M, G_M, SH_F, SC_F, G_F = [slice(i * D, (i + 1) * D) for i in range(6)]

                with ExitStack() as ph:
                    c_sb = sb(ph, "c_sb", [128, 8], F32)
                    sc = sb(ph, "sc", [128, 8], F32)
                    scb = sb(ph, "scb", [128, 8, 128], BF16)
                    wa = Ring([sb(ph, "wa%d" % i, [128, 8, 512], BF16) for i in range(2)])
                    psA = Ring([ps(ph, "psA%d" % i, [128, 512], F32) for i in range(2)])
                    S.dma("sync", lambda e: e.dma_start(out=c_sb[:], in_=cT), writes=[c_sb])
                    S.dma("sync", lambda e: e.dma_start(out=mod_bc[:], in_=b_ada.partition_broadcast(128)), writes=[mod_bc])
                    S.do("scalar", lambda e: e.activation(out=sc[:], in_=c_sb[:], func=AF.Silu), reads=[c_sb], writes=[sc])
                    for k in range(8):
                        S.do("vector", lambda e, k=k: e.tensor_scalar(out=scb[:, k, :], in0=ones_bf[:], scalar1=sc[:, k:k + 1],
                                                                      scalar2=None, op0=ALU.mult),
                             reads=[ones_bf, sc], writes=[scb])
                    for j in range(12):
                        wt = wa.get()
                        S.dma("gpsimd", lambda e, wt=wt, j=j: e.dma_start(out=wt[:], in_=w_ada_v[:, :, j * 512:(j + 1) * 512]),
                              writes=[wt])
                        pt = psA.get()
                        mm(pt, pt[:], [(scb[:, k, :], wt[:, k, :]) for k in range(8)], [scb, wt])
                        S.do("vector", lambda e, pt=pt, j=j: e.tensor_tensor(out=mod_bc[:, j * 512:(j + 1) * 512], in0=pt[:],
                                                                             in1=mod_bc[:, j * 512:(j + 1) * 512], op=ALU.add),
                             reads=[pt], writes=[mod_bc])
                    S.do("vector", lambda e: e.tensor_scalar_add(out=mod_bc[:, SC_M], in0=mod_bc[:, SC_M], scalar1=1.0),
                         writes=[mod_bc])
                    S.do("vector", lambda e: e.tensor_scalar_add(out=mod_bc[:, SC_F], in0=mod_bc[:, SC_F], scalar1=1.0),
                         writes=[mod_bc])
                    S.do("vector", lambda e: e.tensor_copy(out=gf_bc[:], in_=mod_bc[:, G_F]), reads=[mod_bc], writes=[gf_bc])
                    dbg_dump("d_mod", mod_bc, mod_bc[:])
                    S.barrier()
                    if stop == "A":
                        S.dead = True

                with ExitStack() as ph:
                    xin = Ring([sb(ph, "xin%d" % i, [128, D], F32) for i in range(4)])
                    tmpf = Ring([sb(ph, "tmpf%d" % i, [128, D], F32) for i in range(3)])
                    u1b = Ring([sb(ph, "u1b%d" % i, [128, D], BF16) for i in range(3)])
                    ptr = Ring([ps(ph, "ptr%d" % i, [128, 8, 128], BF16) for i in range(2)])
                    for i in range(NT):
                        xi, tf, ub, pt = xin.get(), tmpf.get(), u1b.get(), ptr.get()
                        S.dma("sync", lambda e, xi=xi, i=i: e.dma_start(out=xi[:], in_=x[i * 128:(i + 1) * 128, :]), writes=[xi])
                        S.do("vector", lambda e, xi=xi, tf=tf: e.tensor_tensor(out=tf[:], in0=xi[:], in1=mod_bc[:, SC_M], op=ALU.mult),
                             reads=[xi, mod_bc], writes=[tf])
                        S.do("gpsimd", lambda e, tf=tf, ub=ub: e.tensor_tensor(out=ub[:], in0=tf[:], in1=mod_bc[:, SH_M], op=ALU.add),
                             reads=[tf, mod_bc], writes=[ub])

                        def trf(e, ub=ub, pt=pt):
                            ins = None
                            for k in range(8):
                                ins = e.transpose(out=pt[:, k, :], in_=ub[:, k * 128:(k + 1) * 128], identity=ident_bf[:])
                            return ins
                        S.do("tensor", trf, reads=[ub, ident_bf], writes=[pt])
                        S.do("scalar", lambda e, pt=pt, i=i: e.copy(out=uT[:, :, i * 128:(i + 1) * 128], in_=pt[:]),
                             reads=[pt], writes=[uT_tok[i]])
                    dbg_dump("d_u1T", uT, uT[:].rearrange("p k t -> p (k t)"), BF16)
                    S.barrier()
                    if stop == "B":
                        S.dead = True

                with ExitStack() as phC:
                    obT = sb(phC, "obT", [128, 8, T], BF16)
                    obT_tok = [[Tok() for _ in range(4)] for _ in range(8)]
                    ya1T_tok = [Tok() for _ in range(8)]
                    pb = Ring([ps(phC, "pb%d" % i, [128, 512], F32) for i in range(7)])
                    ptb = ps(phC, "ptb", [128, 4, 128], BF16)

                    with ExitStack() as ph:
                        wal = sb(ph, "wal", [128, 8, 16], BF16)
                        alT = sb(ph, "alT", [16, T], BF16)
                        wau_sb = sb(ph, "wau_sb", [16, 512], BF16)
                        nba = sb(ph, "nba", [128, 4], F32)
                        gain_sb = sb(ph, "gain_sb", [128, 2], F32)
                        whd = Ring([sb(ph, "whd%d" % i, [128, 8, 768], BF16) for i in range(2)])
                        tA = Ring([sb(ph, "tA%d" % i, [128, 512], F32) for i in range(2)])
                        tB = Ring([sb(ph, "tB%d" % i, [128, 512], F32) for i in range(2)])
                        ebt = Ring([sb(ph, "ebt%d" % i, [128, 512], F32) for i in range(2)])
                        enbt = Ring([sb(ph, "enbt%d" % i, [128, 512], F32) for i in range(2)])
                        qsT = Ring([sb(ph, "qsT%d" % i, [128, 512], BF16) for i in range(2)])
                        ksT = Ring([sb(ph, "ksT%d" % i, [128, 512], BF16) for i in range(2)])
                        v_sb = Ring([sb(ph, "v_sb%d" % i, [128, 4, 256], BF16) for i in range(2)])
                        kt_sb = Ring([sb(ph, "kt_sb%d" % i, [128, 4, 128], BF16) for i in range(2)])
                        sT_sb = Ring([sb(ph, "sT_sb%d" % i, [128, 512], BF16) for i in range(2)])
                        Ttmp = Ring([sb(ph, "Ttmp%d" % i, [128, 256], F32) for i in range(2)])
                        Sf = sb(ph, "Sf", [128, 256], F32)
                        Sb = Ring([sb(ph, "Sb%d" % i, [128, 256], BF16) for i in range(2)])
                        oT_sb = Ring([sb(ph, "oT_sb%d" % i, [128, 2, 512], F32) for i in range(1)])
                        sq = Ring([sb(ph, "sq%d" % i, [128, 2, 512], BF16) for i in range(1)])
                        rstd = Ring([sb(ph, "rstd%d" % i, [128, 512], F32) for i in range(2)])
                        srt = Ring([sb(ph, "srt%d" % i, [128, 512], F32) for i in range(2)])
                        zb = Ring([sb(ph, "zb%d" % i, [128, 512], F32) for i in range(2)])

                        zt4 = sb(ph, "zt4", [128, 2 * D], BF16)
                        S.do("gpsimd", lambda e: e.memset(zt4[:], 0.0), writes=[zt4])
                        for zi in range(NROWS // 256):
                            S.dma("sync", lambda e, zi=zi: e.dma_start(out=xdisp[zi * 256:(zi + 1) * 256, :].rearrange("(p a) d -> p (a d)", p=128),
                                                                      in_=zt4[:]), reads=[zt4], writes=[xz_tok.get()])
                        S.dma("gpsimd", lambda e: e.dma_start(out=wal[:], in_=w_in_v[:, :, 4096:4112]), writes=[wal])
                        S.dma("gpsimd", lambda e: e.dma_start(out=wau_sb[:], in_=wau), writes=[wau_sb])
                        S.dma("sync", lambda e: e.dma_start(out=nba[:], in_=baT), writes=[nba])
                        S.dma("sync", lambda e: e.dma_start(out=gain_sb[:], in_=gainT), writes=[gain_sb])
                        S.do("vector", lambda e: e.tensor_scalar(out=nba[:], in0=nba[:], scalar1=-1.0, scalar2=None, op0=ALU.mult),
                             writes=[nba])
                        for tt in range(4):
                            pt = pb.get()
                            mm(pt, pt[0:16, :], [(wal[:, k, :], uT[:, k, tt * 512:(tt + 1) * 512]) for k in range(8)],
                               [wal] + uT_reads(tt))
                            S.do("scalar", lambda e, pt=pt, tt=tt: e.copy(out=alT[:, tt * 512:(tt + 1) * 512], in_=pt[0:16, :]),
                                 reads=[pt], writes=[alT])

                        if stop == "C1":
                            S.dead = True
                        def load_head(h):
                            wh_ = whd.get()
                            S.dma("gpsimd", [(lambda e, wh_=wh_, c0=c0, n=n, o=o: e.dma_start(out=wh_[:, :, o:o + n], in_=w_in_v[:, :, c0:c0 + n]))
                                             for (c0, n, o) in [(1024 + h * 128, 128, 0), (1536 + h * 128, 128, 128),
                                                                (3072 + h * 256, 256, 256), (2048 + h * 256, 256, 512)]], writes=[wh_])
                            return wh_
                        wh_next = load_head(0)
                        for h in range(4):
                            wh = wh_next
                            if h + 1 < 4:
                                wh_next = load_head(h + 1)
                            s_prev = None
                            for tt in range(4):
                                tsl = slice(tt * 512, (tt + 1) * 512)
                                ur = uT_reads(tt)
                                bz = pb.get()
                                mm(bz, bz[:], [(wau_sb[:, h * 128:(h + 1) * 128], alT[:, tsl])], [wau_sb, alT])
                                a1, b1, eb, enb = tA.get(), tB.get(), ebt.get(), enbt.get()
                                l1 = a1
                                S.do("scalar", lambda e, bz=bz, a1=a1, h=h: e.activation(out=a1[:], in_=bz[:], func=AF.Exp,
                                                                                         bias=nba[:, h:h + 1], scale=-1.0),
                                     reads=[bz, nba], writes=[a1])
                                S.do("scalar", lambda e, a1=a1: e.activation(out=a1[:], in_=a1[:], func=AF.Ln, bias=1.0),
                                     writes=[a1])
                                S.do("vector", lambda e, l1=l1, b1=b1: e.tensor_tensor_scan(out=b1[:], data0=mask01[:], data1=l1[:],
                                                                                            initial=0.0, op0=ALU.mult, op1=ALU.add),
                                     reads=[l1, mask01], writes=[b1])
                                S.do("scalar", lambda e, b1=b1, eb=eb: e.activation(out=eb[:], in_=b1[:], func=AF.Exp, scale=-1.0 / 16.0),
                                     reads=[b1], writes=[eb])
                                S.do("scalar", lambda e, b1=b1, enb=enb: e.activation(out=enb[:], in_=b1[:], func=AF.Exp, scale=1.0 / 16.0),
                                     reads=[b1], writes=[enb])
                                if stop == "C2a":
                                    S.dead = True
                                bq = pb.get()
                                mm(bq, bq[:], [(wh[:, k, 0:128], uT[:, k, tsl]) for k in range(8)], [wh] + ur)
                                qs = qsT.get()
                                S.do("vector", lambda e, bq=bq, eb=eb, qs=qs: e.scalar_tensor_tensor(
                                    out=qs[:], in0=bq[:], scalar=128.0 ** -0.5, in1=eb[:], op0=ALU.mult, op1=ALU.mult),
                                    reads=[bq, eb], writes=[qs])
                                bk = pb.get()
                                mm(bk, bk[:], [(wh[:, k, 128:256], uT[:, k, tsl]) for k in range(8)], [wh] + ur)
                                ks = ksT.get()
                                S.do("vector", lambda e, bk=bk, enb=enb, ks=ks: e.tensor_tensor(out=ks[:], in0=bk[:], in1=enb[:], op=ALU.mult),
                                     reads=[bk, enb], writes=[ks])
                                vs = v_sb.get()
                                for half in range(2):
                                    bv = pb.get()

                                    def vfn(e, bv=bv, half=half, tt=tt, wh=wh):
                                        ins = None
                                        for jj in range(2):
                                            j = half * 2 + jj
                                            t0 = (tt * 4 + j) * 128
                                            for k in range(8):
                                                ins = e.matmul(bv[:, jj * 256:(jj + 1) * 256], lhsT=uT[:, k, t0:t0 + 128],
                                                               rhs=wh[:, k, 512:768], start=(k == 0), stop=(k == 7))
                                        return ins
                                    S.do("tensor", vfn, reads=[wh] + ur, writes=[bv])
                                    S.do("scalar", lambda e, bv=bv, vs=vs, half=half: e.copy(
                                        out=vs[:, half * 2:half * 2 + 2, :], in_=bv[:].rearrange("p (j n) -> p j n", j=2)),
                                        reads=[bv], writes=[vs])
                                if stop == "C2b":
                                    S.dead = True

                                def ktf(e, ks=ks):
                                    ins = None
                                    for j in range(4):
                                        ins = e.transpose(out=ptb[:, j, :], in_=ks[:, j * 128:(j + 1) * 128], identity=ident_bf[:])
                                    return ins
                                S.do("tensor", ktf, reads=[ks, ident_bf], writes=[ptb])
                                kt = kt_sb.get()
                                S.do("vector", lambda e, kt=kt: e.tensor_copy(out=kt[:], in_=ptb[:]), reads=[ptb], writes=[kt])
                                bs = pb.get()

                                def scf(e, bs=bs, ks=ks, qs=qs):
                                    ins = None
                                    for j in range(4):
                                        js = slice(j * 128, (j + 1) * 128)
                                        ins = e.matmul(bs[:, js], lhsT=ks[:, js], rhs=qs[:, js], start=True, stop=True)
                                    return ins
                                S.do("tensor", scf, reads=[ks, qs], writes=[bs])
                                sT = sT_sb.get()
                                S.do("vector", lambda e, bs=bs, sT=sT: e.tensor_tensor(out=sT[:], in0=bs[:], in1=cmask4[:], op=ALU.mult),
                                     reads=[bs, cmask4], writes=[sT])
                                bd = [pb.get(), pb.get()]
                                for half in range(2):
                                    def dsf(e, bdh=bd[half], half=half, kt=kt, vs=vs):
                                        ins = None
                                        for jj in range(2):
                                            j = half * 2 + jj
                                            ins = e.matmul(bdh[:, jj * 256:(jj + 1) * 256], lhsT=kt[:, j, :], rhs=vs[:, j, :],
                                                           start=True, stop=True)
                                        return ins
                                    S.do("tensor", dsf, reads=[kt, vs], writes=[bd[half]])
                                if stop == "C2c":
                                    S.dead = True
                                bo = [pb.get(), pb.get()]
                                for j in range(4):
                                    c = tt * 4 + j
                                    js = slice(j * 128, (j + 1) * 128)
                                    for ec in range(2):
                                        def of(e, boe=bo[ec], ec=ec, j=j, js=js, c=c, vs=vs, sT=sT, qs=qs, s_prev=s_prev):
                                            ins = e.matmul(boe[:, js], lhsT=vs[:, j, ec * 128:(ec + 1) * 128], rhs=sT[:, js],
                                                           start=True, stop=(c == 0))
                                            if c > 0:
                                                ins = e.matmul(boe[:, js], lhsT=s_prev[:, ec * 128:(ec + 1) * 128], rhs=qs[:, js],
                                                               start=False, stop=True)
                                            return ins
                                        S.do("tensor", of, reads=[vs, sT, qs] + ([s_prev] if c > 0 else []), writes=[bo[ec]])
                                    bdh = bd[j // 2]
                                    dsl = slice((j % 2) * 256, (j % 2 + 1) * 256)
                                    tm = Ttmp.get()
                                    if c == 0:
                                        S.do("vector", lambda e, bdh=bdh, dsl=dsl, tm=tm: e.tensor_copy(out=tm[:], in_=bdh[:, dsl]),
                                             reads=[bdh], writes=[tm])
                                    else:
                                        S.do("vector", lambda e, bdh=bdh, dsl=dsl, tm=tm: e.tensor_tensor(out=tm[:], in0=bdh[:, dsl], in1=Sf[:], op=ALU.add),
                                             reads=[bdh, Sf], writes=[tm])
                                    col = j * 128 + 127
                                    sbn = Sb.get()
                                    S.do("scalar", lambda e, tm=tm, eb=eb, col=col: e.activation(out=Sf[:], in_=tm[:], func=AF.Identity,
                                                                                                scale=eb[:, col:col + 1]),
                                         reads=[tm, eb], writes=[Sf])
                                    S.do("scalar", lambda e, tm=tm, eb=eb, col=col, sbn=sbn: e.activation(out=sbn[:], in_=tm[:], func=AF.Identity,
                                                                                                         scale=eb[:, col:col + 1]),
                                         reads=[tm, eb], writes=[sbn])
                                    s_prev = sbn
                                if stop == "C2d":
                                    S.dead = True
                                ot, sqq = oT_sb.get(), sq.get()
                                for ec in range(2):
                                    S.do("vector", lambda e, ot=ot, ec=ec, boe=bo[ec]: e.tensor_copy(out=ot[:, ec, :], in_=boe[:]),
                                         reads=[bo[ec]], writes=[ot])
                                    S.do("gpsimd", lambda e, sqq=sqq, ec=ec, ot=ot: e.tensor_tensor(out=sqq[:, ec, :], in0=ot[:, ec, :], in1=ot[:, ec, :], op=ALU.mult),
                                         reads=[ot], writes=[sqq])
                                bss = pb.get()
                                mm(bss, bss[:], [(ones_bf[:], sqq[:, ec, :]) for ec in range(2)], [ones_bf, sqq])
                                if stop == "C2e0":
                                    S.dead = True
                                rs = rstd.get()
                                S.do("vector", lambda e, bss=bss, rs=rs: e.tensor_scalar(out=rs[:], in0=bss[:], scalar1=1.0 / 256.0, scalar2=1e-6,
                                                                                         op0=ALU.mult, op1=ALU.add),
                                     reads=[bss], writes=[rs])
                                S.do("scalar", lambda e, rs=rs: e.activation(out=rs[:], in_=rs[:], func=AF.Sqrt), writes=[rs])
                                S.do("vector", lambda e, rs=rs: e.reciprocal(out=rs[:], in_=rs[:]), writes=[rs])
                                if stop == "C2e1":
                                    S.dead = True
                                for ec in range(2):
                                    brr = pb.get()
                                    mm(brr, brr[:], [(wh[:, k, 256 + ec * 128:256 + (ec + 1) * 128], uT[:, k, tsl]) for k in range(8)],
                                       [wh] + ur)
                                    sr = srt.get()
                                    S.do("scalar", lambda e, brr=brr, sr=sr: e.activation(out=sr[:], in_=brr[:], func=AF.Silu),
                                         reads=[brr], writes=[sr])
                                    if stop == "C2e2":
                                        S.dead = True
                                    z1 = zb.get()
                                    S.do("vector", lambda e, ot=ot, ec=ec, rs=rs, z1=z1: e.scalar_tensor_tensor(
                                        out=z1[:], in0=ot[:, ec, :], scalar=gain_sb[:, ec:ec + 1], in1=rs[:], op0=ALU.mult, op1=ALU.mult),
                                        reads=[ot, gain_sb, rs], writes=[z1])
                                    S.do("gpsimd", lambda e, z1=z1, sr=sr, h=h, ec=ec, tsl=tsl: e.tensor_tensor(
                                        out=obT[:, 2 * h + ec, tsl], in0=z1[:], in1=sr[:], op=ALU.mult),
                                        reads=[z1, sr], writes=[obT_tok[2 * h + ec][tt]])
                                if stop == "C2e":
                                    S.dead = True
                            if stop == "C2f":
                                S.dead = True
                        S.barrier()
                    dbg_dump("d_obT", obT, obT[:].rearrange("p k t -> p (k t)"), BF16)
                    if stop == "C2":
                        S.dead = True

                    with ExitStack() as ph34:
                        ya1T = sb(ph34, "ya1T", [128, 8, T], BF16)
                        with ExitStack() as ph:
                            wblk = Ring([sb(ph, "wblk%d" % i, [128, 8, 256], BF16) for i in range(2)])
                            wpg_sb = Ring([sb(ph, "wpg_sb%d" % i, [128, 2, 256], BF16) for i in range(2)])
                            psc_sb = sb(ph, "psc_sb", [128, 8], F32)
                            aT = Ring([sb(ph, "aT%d" % i, [128, T], F32) for i in range(2)])
                            sA = sb(ph, "sA", [128, T], F32)
                            sB = sb(ph, "sB", [128, T], F32)
                            pooled = Ring([sb(ph, "pooled%d" % i, [128, 2, T], BF16) for i in range(2)])
                            t15 = sb(ph, "t15", [128, 16], F32)
                            S.dma("sync", lambda e: e.dma_start(out=psc_sb[:], in_=pscT), writes=[psc_sb])
                            def load_grp(g):
                                wb_, wp_ = wblk.get(), wpg_sb.get()
                                S.dma("gpsimd", lambda e, wb_=wb_, g=g: e.dma_start(out=wb_[:], in_=w_in_v[:, :, g * 256:(g + 1) * 256]), writes=[wb_])
                                S.dma("gpsimd", lambda e, wp_=wp_, g=g: e.dma_start(out=wp_[:], in_=wpg[g].rearrange("(k p) n -> p k n", p=128)), writes=[wp_])
                                return wb_, wp_
                            grp_next = load_grp(0)
                            for g in range(4):
                                wb, wp = grp_next
                                pl = pooled.get()
                                if g + 1 < 4:
                                    grp_next = load_grp(g + 1)
                                w = 2 ** (g + 1)
                                for cc in range(2):
                                    a = aT.get()
                                    for tt in range(4):
                                        tsl = slice(tt * 512, (tt + 1) * 512)
                                        pt = pb.get()
                                        mm(pt, pt[:], [(wb[:, k, cc * 128:(cc + 1) * 128], uT[:, k, tsl]) for k in range(8)], [wb] + uT_reads(tt))
                                        S.do("scalar", lambda e, pt=pt, a=a, tsl=tsl: e.copy(out=a[:, tsl], in_=pt[:]), reads=[pt], writes=[a])
                                    src = a
                                    for stp in range(g + 1):
                                        sh = 2 ** stp
                                        dst = sA if (stp % 2 == 0) else sB
                                        S.do("vector", lambda e, src=src, dst=dst, sh=sh: e.tensor_tensor(out=dst[:, sh:], in0=src[:, sh:], in1=src[:, :T - sh], op=ALU.add),
                                             reads=[src], writes=[dst])
                                        S.do("gpsimd", lambda e, src=src, dst=dst, sh=sh: e.tensor_copy(out=dst[:, 0:sh], in_=src[:, 0:sh]),
                                             reads=[src], writes=[dst])
                                        src = dst
                                    S.do("vector", lambda e, src=src, a=a, pl=pl, cc=cc, w=w: e.scalar_tensor_tensor(
                                        out=pl[:, cc, :], in0=src[:], scalar=1.0 / w, in1=a[:], op0=ALU.mult, op1=ALU.subtract),
                                        reads=[src, a], writes=[pl])
                                    S.do("vector", lambda e, src=src, w=w: e.tensor_tensor(out=t15[:, 0:w - 1], in0=src[:, 0:w - 1], in1=rc16[:, 0:w - 1], op=ALU.mult),
                                         reads=[src, rc16], writes=[t15])
                                    S.do("vector", lambda e, a=a, pl=pl, cc=cc, w=w: e.tensor_tensor(out=pl[:, cc, 0:w - 1], in0=t15[:, 0:w - 1], in1=a[:, 0:w - 1], op=ALU.subtract),
                                         reads=[t15, a], writes=[pl])
                                for mo in range(2):
                                    for tt in range(4):
                                        tsl = slice(tt * 512, (tt + 1) * 512)
                                        pt = pb.get()
                                        mm(pt, pt[:], [(wp[:, kc, mo * 128:(mo + 1) * 128], pl[:, kc, tsl]) for kc in range(2)], [wp, pl])
                                        S.do("vector", lambda e, pt=pt, g=g, mo=mo, tsl=tsl: e.tensor_scalar(
                                            out=ya1T[:, 2 * g + mo, tsl], in0=pt[:], scalar1=psc_sb[:, 2 * g + mo:2 * g + mo + 1], scalar2=None, op0=ALU.mult),
                                            reads=[pt, psc_sb], writes=[ya1T_tok[2 * g + mo]])
                            S.barrier()
                        dbg_dump("d_ya1T", ya1T, ya1T[:].rearrange("p k t -> p (k t)"), BF16)
                        if stop == "C3":
                            S.dead = True


                        with ExitStack() as ph:
                            wg4 = Ring([sb(ph, "wg4_%d" % i, [128, 4, 8, 256], BF16) for i in range(2)])
                            sga = Ring([sb(ph, "sga%d" % i, [128, 512], F32) for i in range(2)])
                            sgb = Ring([sb(ph, "sgb%d" % i, [128, 512], F32) for i in range(2)])
                            m1 = Ring([sb(ph, "m1_%d" % i, [128, 512], F32) for i in range(2)])
                            m2 = Ring([sb(ph, "m2_%d" % i, [128, 512], F32) for i in range(2)])
                            stg = sb(ph, "stg", [128, 8, 512], BF16)
                            for tt in range(4):
                                tsl = slice(tt * 512, (tt + 1) * 512)
                                ur = uT_reads(tt)
                                obr = [obT_tok[k][tt] for k in range(8)]
                                for mo in range(8):
                                    if mo % 2 == 0:
                                        wg = wg4.get()
                                        q0 = (mo // 2) * 256
                                        srcs = [w_in_v[:, :, 4112 + q0:4112 + q0 + 256], w_in_v[:, :, 5136 + q0:5136 + q0 + 256],
                                                wba_v[:, :, q0:q0 + 256], wbb_v[:, :, q0:q0 + 256]]
                                        S.dma("gpsimd", [(lambda e, wg=wg, i4=i4, srcs=srcs: e.dma_start(out=wg[:, i4, :, :], in_=srcs[i4])) for i4 in range(4)],
                                              writes=[wg])
                                    mq = slice((mo % 2) * 128, (mo % 2 + 1) * 128)
                                    bga = pb.get()
                                    mm(bga, bga[:], [(wg[:, 0, k, mq], uT[:, k, tsl]) for k in range(8)], [wg] + ur)
                                    ga = sga.get()
                                    S.do("scalar", lambda e, bga=bga, ga=ga: e.activation(out=ga[:], in_=bga[:], func=AF.Sigmoid), reads=[bga], writes=[ga])
                                    bgb = pb.get()
                                    mm(bgb, bgb[:], [(wg[:, 1, k, mq], uT[:, k, tsl]) for k in range(8)], [wg] + ur)
                                    gb = sgb.get()
                                    S.do("scalar", lambda e, bgb=bgb, gb=gb: e.activation(out=gb[:], in_=bgb[:], func=AF.Sigmoid), reads=[bgb], writes=[gb])
                                    bya = pb.get()
                                    mm(bya, bya[:], [(wg[:, 2, k, mq], ya1T[:, k, tsl]) for k in range(8)], [wg] + ya1T_tok)
                                    mm1 = m1.get()
                                    S.do("vector", lambda e, bya=bya, ga=ga, mm1=mm1: e.tensor_tensor(out=mm1[:], in0=bya[:], in1=ga[:], op=ALU.mult),
                                         reads=[bya, ga], writes=[mm1])
                                    byb = pb.get()
                                    mm(byb, byb[:], [(wg[:, 3, k, mq], obT[:, k, tsl]) for k in range(8)], [wg] + obr)
                                    mm2 = m2.get()
                                    S.do("vector", lambda e, byb=byb, gb=gb, mm2=mm2: e.tensor_tensor(out=mm2[:], in0=byb[:], in1=gb[:], op=ALU.mult),
                                         reads=[byb, gb], writes=[mm2])
                                    S.do("vector", lambda e, mm1=mm1, mm2=mm2, mo=mo: e.tensor_tensor(out=stg[:, mo, :], in0=mm1[:], in1=mm2[:], op=ALU.add),
                                         reads=[mm1, mm2], writes=[stg])
                                S.do("scalar", lambda e, tsl=tsl: e.copy(out=obT[:, :, tsl], in_=stg[:]), reads=[stg], writes=obr)
                            S.barrier()
                    mTb = obT
                    mTb_tok = [Tok() for _ in range(NT // 4)]
                    dbg_dump("d_mT", mTb, mTb[:].rearrange("p k t -> p (k t)"), BF16)
                    if stop == "C4":
                        S.dead = True

                    with ExitStack() as phD:
                        wout_sb = sb(phD, "wout_sb", [128, 8, D], BF16)
                        g1_bc = sb(phD, "g1_bc", [128, D], F32)
                        b1_bc = sb(phD, "b1_bc", [128, D], F32)
                        wr_sb = sb(phD, "wr_sb", [128, 8, NE], F32)
                        br_bc = sb(phD, "br_bc", [128, NE], F32)
                        xin = Ring([sb(phD, "xinD%d" % i, [128, D], F32) for i in range(2)])
                        t1 = Ring([sb(phD, "t1_%d" % i, [128, D], F32) for i in range(2)])
                        zt = Ring([sb(phD, "zt%d" % i, [128, D], F32) for i in range(2)])
                        x1t = Ring([sb(phD, "x1t%d" % i, [128, D], F32) for i in range(2)])
                        u2t = Ring([sb(phD, "u2t%d" % i, [128, D], F32) for i in range(4)])
                        u2Tf = Ring([sb(phD, "u2Tf%d" % i, [128, 8, 128], F32) for i in range(4)])
                        stats = Ring([sb(phD, "stats%d" % i, [128, 2, 6], F32) for i in range(2)])
                        mv = Ring([sb(phD, "mv%d" % i, [128, 2], F32) for i in range(2)])
                        rsd = Ring([sb(phD, "rsd%d" % i, [128, 1], F32) for i in range(2)])
                        lg = Ring([sb(phD, "lg%d" % i, [128, NE], F32) for i in range(2)])
                        top8 = Ring([sb(phD, "top8_%d" % i, [128, 8], F32) for i in range(2)])
                        nmx = Ring([sb(phD, "nmx%d" % i, [128, 1], F32) for i in range(2)])
                        msk = Ring([sb(phD, "msk%d" % i, [128, NE], F32) for i in range(2)])
                        ex = Ring([sb(phD, "ex%d" % i, [128, NE], F32) for i in range(2)])
                        ssum = Ring([sb(phD, "ssum%d" % i, [128, 1], F32) for i in range(2)])
                        wtk = Ring([sb(phD, "wtk%d" % i, [128, NE], F32) for i in range(2)])
                        mkb_r = Ring([sb(phD, "mkb%d" % i, [128, NE], BF16) for i in range(2)])
                        i8_r = Ring([sb(phD, "i8_%d" % i, [128, 8], U32) for i in range(2)])
                        e4_r = Ring([sb(phD, "e4_%d" % i, [128, 4], F32) for i in range(2)])
                        ek_r = Ring([sb(phD, "ek_%d" % i, [128, 4], F32) for i in range(2)])
                        w4_r = Ring([sb(phD, "w4_%d" % i, [128, 4], F32) for i in range(2)])
                        pos_r = Ring([sb(phD, "pos_%d" % i, [128, NE], F32) for i in range(2)])
                        p4_r = Ring([sb(phD, "p4_%d" % i, [128, 4], F32) for i in range(2)])
                        oh_r = Ring([sb(phD, "oh_%d" % i, [128, NE], F32) for i in range(2)])
                        sl_r = Ring([sb(phD, "sl_%d" % i, [128, 4], F32) for i in range(2)])
                        ov_r = Ring([sb(phD, "ov_%d" % i, [128, 4], F32) for i in range(2)])
                        scf_r = Ring([sb(phD, "scf_%d" % i, [128, 4], F32) for i in range(2)])
                        sci_r = Ring([sb(phD, "sci_%d" % i, [128, 4], I32) for i in range(2)])
                        kp_r = Ring([sb(phD, "kp_%d" % i, [128, 4], F32) for i in range(2)])
                        u2b_r = Ring([sb(phD, "u2b_%d" % i, [128, D], BF16) for i in range(2)])
                        cnt_bc = sb(phD, "cnt_bc", [128, NE], F32)
                        S.do("gpsimd", lambda e: e.memset(cnt_bc[:], 0.0), writes=[cnt_bc])
                        gidx_tok, w4_tok = Tok(), Tok()
                        x1s_tok = [Tok() for _ in range(NT)]
                        for n in range(2):
                            S.dma("gpsimd", lambda e, n=n: e.dma_start(out=wout_sb[:, :, n * 512:(n + 1) * 512], in_=wout_v[:, :, n * 512:(n + 1) * 512]),
                                  writes=[wout_sb])
                        S.dma("sync", lambda e: e.dma_start(out=g1_bc[:], in_=ln1g.partition_broadcast(128)), writes=[g1_bc])
                        S.dma("sync", lambda e: e.dma_start(out=b1_bc[:], in_=ln1b.partition_broadcast(128)), writes=[b1_bc])
                        S.dma("sync", lambda e: e.dma_start(out=wr_sb[:], in_=wr_v), writes=[wr_sb])
                        S.dma("sync", lambda e: e.dma_start(out=br_bc[:], in_=br.partition_broadcast(128)), writes=[br_bc])

                        def layer_norm(zz, outt, gbc, bbc, st_, mv_, rs_, eng_gb="gpsimd", act_norm=False):
                            for c2 in range(2):
                                S.do("vector", lambda e, c2=c2: e.bn_stats(out=st_[:, c2, :], in_=zz[:, c2 * 512:(c2 + 1) * 512]),
                                     reads=[zz], writes=[st_])
                            S.do("vector", lambda e: e.bn_aggr(out=mv_[:], in_=st_[:].rearrange("p a b -> p (a b)")), reads=[st_], writes=[mv_])
                            S.do("vector", lambda e: e.tensor_scalar_add(out=rs_[:], in0=mv_[:, 1:2], scalar1=1e-5), reads=[mv_], writes=[rs_])
                            S.do("scalar", lambda e: e.activation(out=rs_[:], in_=rs_[:], func=AF.Sqrt), writes=[rs_])
                            S.do("vector", lambda e: e.reciprocal(out=rs_[:], in_=rs_[:]), writes=[rs_])
                            if act_norm:
                                S.do("vector", lambda e: e.scalar_tensor_tensor(out=mv_[:, 1:2], in0=mv_[:, 0:1], scalar=-1.0, in1=rs_[:, 0:1],
                                                                                op0=ALU.mult, op1=ALU.mult), reads=[rs_], writes=[mv_])
                                S.do("scalar", lambda e: e.activation(out=zz[:], in_=zz[:], func=AF.Identity, bias=mv_[:, 1:2], scale=rs_[:, 0:1]),
                                     reads=[mv_, rs_], writes=[zz])
                            else:
                                S.do("vector", lambda e: e.tensor_scalar(out=zz[:], in0=zz[:], scalar1=mv_[:, 0:1], scalar2=rs_[:, 0:1],
                                                                         op0=ALU.subtract, op1=ALU.mult),
                                     reads=[mv_, rs_], writes=[zz])
                            S.do(eng_gb, lambda e: e.tensor_tensor(out=zz[:], in0=zz[:], in1=gbc[:], op=ALU.mult), reads=[gbc], writes=[zz])
                            S.do(eng_gb, lambda e: e.tensor_tensor(out=outt[:], in0=zz[:], in1=bbc[:], op=ALU.add), reads=[zz, bbc], writes=[outt])

                        def stage1(i):
                            isl = slice(i * 128, (i + 1) * 128)
                            by = [pb.get(), pb.get()]
                            for n in range(2):
                                mm(by[n], by[n][:], [(mTb[:, k, isl], wout_sb[:, k, n * 512:(n + 1) * 512]) for k in range(8)],
                                   [mTb_tok[i // 4], wout_sb])
                            xi, tt1, zz, x1, u2 = xin.get(), t1.get(), zt.get(), x1t.get(), u2t.get()
                            S.dma("sync", lambda e, xi=xi, isl=isl: e.dma_start(out=xi[:], in_=x[isl, :]), writes=[xi])
                            for n in range(2):
                                nsl = slice(n * 512, (n + 1) * 512)
                                S.do("vector", lambda e, n=n, nsl=nsl, tt1=tt1, byn=by[n]: e.tensor_tensor(
                                    out=tt1[:, nsl], in0=byn[:], in1=mod_bc[:, 2 * D + n * 512:2 * D + (n + 1) * 512], op=ALU.mult),
                                    reads=[by[n], mod_bc], writes=[tt1])
                            S.do("vector", lambda e, xi=xi, tt1=tt1, zz=zz: e.scalar_tensor_tensor(out=zz[:], in0=xi[:], scalar=ALPHA, in1=tt1[:],
                                                                                                  op0=ALU.mult, op1=ALU.add),
                                 reads=[xi, tt1], writes=[zz])
                            layer_norm(zz, x1, g1_bc, b1_bc, stats.get(), mv.get(), rsd.get())
                            if stop == "D1":
                                S.dead = True
                            S.dma("sync", lambda e, x1=x1, isl=isl: e.dma_start(out=x1s[isl, :], in_=x1[:]), reads=[x1], writes=[x1s_tok[i]])
                            if dbg:
                                S.dma("sync", lambda e, x1=x1, isl=isl: e.dma_start(out=dbg_out["d_x1"][isl, :], in_=x1[:]), reads=[x1], writes=[Tok()])
                            S.do("vector", lambda e, x1=x1, u2=u2: e.tensor_tensor(out=u2[:], in0=x1[:], in1=mod_bc[:, SC_F], op=ALU.mult),
                                 reads=[x1, mod_bc], writes=[u2])
                            S.do("gpsimd", lambda e, u2=u2: e.tensor_tensor(out=u2[:], in0=u2[:], in1=mod_bc[:, SH_F], op=ALU.add),
                                 reads=[mod_bc], writes=[u2])
                            if stop == "D2":
                                S.dead = True
                            bt = [pb.get(), pb.get()]
                            for n in range(2):
                                def trf(e, n=n, u2=u2, btn=bt[n]):
                                    ins = None
                                    for kk in range(4):
                                        k = n * 4 + kk
                                        ins = e.transpose(out=btn[:, kk * 128:(kk + 1) * 128], in_=u2[:, k * 128:(k + 1) * 128], identity=ident_f[:])
                                    return ins
                                S.do("tensor", trf, reads=[u2, ident_f], writes=[bt[n]])
                            if stop == "D2a":
                                S.dead = True
                            uf = u2Tf.get()
                            for n in range(2):
                                S.do("scalar", lambda e, n=n, uf=uf, btn=bt[n]: e.copy(out=uf[:, n * 4:(n + 1) * 4, :], in_=btn[:].rearrange("p (k m) -> p k m", k=4)),
                                     reads=[bt[n]], writes=[uf])
                                if stop == "D2b":
                                    S.dead = True
                            if stop == "D3":
                                S.dead = True
                            return isl, u2, uf

                        def stage2(i, isl, u2, uf):
                            bl = pb.get()
                            mm(bl, bl[:, 0:NE], [(uf[:, k, :], wr_sb[:, k, :]) for k in range(8)], [uf, wr_sb])
                            yield
                            l_, t8, nm, ss = lg.get(), top8.get(), nmx.get(), ssum.get()
                            S.do("vector", lambda e, bl=bl, l_=l_: e.tensor_tensor(out=l_[:], in0=bl[:, 0:NE], in1=br_bc[:], op=ALU.add),
                                 reads=[bl, br_bc], writes=[l_])
                            yield
                            S.do("vector", lambda e, l_=l_, t8=t8: e.max(out=t8[:], in_=l_[:]), reads=[l_], writes=[t8])
                            yield
                            S.do("vector", lambda e, t8=t8, nm=nm: e.tensor_scalar(out=nm[:], in0=t8[:, 0:1], scalar1=-1.0, scalar2=None, op0=ALU.mult),
                                 reads=[t8], writes=[nm])
                            yield
                            mkb, i8, e4, ek, w4t = mkb_r.get(), i8_r.get(), e4_r.get(), ek_r.get(), w4_r.get()
                            S.do("vector", lambda e, l_=l_, t8=t8, mkb=mkb: e.tensor_scalar(out=mkb[:], in0=l_[:], scalar1=t8[:, 3:4], scalar2=None, op0=ALU.is_ge),
                                 reads=[l_, t8], writes=[mkb])
                            yield
                            S.do("vector", lambda e, l_=l_, t8=t8, i8=i8: e.max_index(out=i8[:], in_max=t8[:], in_values=l_[:]), reads=[l_, t8], writes=[i8])
                            yield
                            S.do("vector", lambda e, i8=i8, e4=e4: e.tensor_copy(out=e4[:], in_=i8[:, 0:4]), reads=[i8], writes=[e4])
                            yield
                            S.do("scalar", lambda e, t8=t8, nm=nm, ek=ek: e.activation(out=ek[:], in_=t8[:, 0:4], func=AF.Exp, bias=nm[:, 0:1], scale=1.0),
                                 reads=[t8, nm], writes=[ek])
                            yield
                            S.do("vector", lambda e, ek=ek, ss=ss: e.reduce_sum(out=ss[:], in_=ek[:], axis=AX.X), reads=[ek], writes=[ss])
                            yield
                            S.do("vector", lambda e, ss=ss: e.reciprocal(out=ss[:], in_=ss[:]), writes=[ss])
                            yield
                            S.do("vector", lambda e, ek=ek, ss=ss, w4t=w4t: e.tensor_scalar(out=w4t[:], in0=ek[:], scalar1=ss[:, 0:1], scalar2=None, op0=ALU.mult),
                                 reads=[ek, ss], writes=[w4t])
                            yield
                            pp = pb.get()

                            def ppf(e, pp=pp, mkb=mkb):
                                e.matmul(pp[:, 0:NE], lhsT=Ltri[:], rhs=mkb[:], start=True, stop=True)
                                return e.matmul(pp[:, NE:2 * NE], lhsT=ones_bf[:], rhs=mkb[:], start=True, stop=True)
                            S.do("tensor", ppf, reads=[Ltri, ones_bf, mkb], writes=[pp])
                            yield
                            pos, p4, sl, ov, scf, sci, kp = pos_r.get(), p4_r.get(), sl_r.get(), ov_r.get(), scf_r.get(), sci_r.get(), kp_r.get()
                            S.do("vector", lambda e, pp=pp, pos=pos: e.tensor_tensor(out=pos[:], in0=pp[:, 0:NE], in1=cnt_bc[:], op=ALU.add),
                                 reads=[pp, cnt_bc], writes=[pos])
                            yield
                            S.do("vector", lambda e, pp=pp: e.tensor_tensor(out=cnt_bc[:], in0=pp[:, NE:2 * NE], in1=cnt_bc[:], op=ALU.add),
                                 reads=[pp], writes=[cnt_bc])
                            yield
                            for k4 in range(4):
                                oh = oh_r.get()
                                S.do("vector", lambda e, oh=oh, e4=e4, k4=k4: e.tensor_scalar(out=oh[:], in0=iota32[:], scalar1=e4[:, k4:k4 + 1], scalar2=None, op0=ALU.is_equal),
                                     reads=[iota32, e4], writes=[oh])
                                yield
                                S.do("vector", lambda e, oh=oh, pos=pos: e.tensor_tensor(out=oh[:], in0=oh[:], in1=pos[:], op=ALU.mult), reads=[pos], writes=[oh])
                                yield
                                S.do("vector", lambda e, oh=oh, p4=p4, k4=k4: e.reduce_sum(out=p4[:, k4:k4 + 1], in_=oh[:], axis=AX.X), reads=[oh], writes=[p4])
                                yield
                            S.do("vector", lambda e, e4=e4, i=i: e.tensor_copy(out=e4_all[:, i * 4:(i + 1) * 4], in_=e4[:]), reads=[e4], writes=[e4a_tok])
                            yield
                            S.do("vector", lambda e, p4=p4, i=i: e.tensor_copy(out=p4_all[:, i * 4:(i + 1) * 4], in_=p4[:]), reads=[p4], writes=[p4a_tok])
                            yield
                            S.do("vector", lambda e, w4t=w4t, i=i: e.tensor_copy(out=w4_all[:, i * 4:(i + 1) * 4], in_=w4t[:]), reads=[w4t], writes=[w4_tok])
                            yield
                            u2b = u2b_r.get()
                            S.do("gpsimd", lambda e, u2=u2, u2b=u2b: e.tensor_copy(out=u2b[:], in_=u2[:]), reads=[u2], writes=[u2b])
                            yield
                            S.dma("sync", lambda e, u2b=u2b, isl=isl: e.dma_start(out=u2s[isl, :], in_=u2b[:]), reads=[u2b], writes=[u2s_tok.get()])
                            yield
                        def run2(ga, gb, stagger=3):
                            live_a, live_b, n = True, True, 0
                            while live_a or live_b:
                                if live_a:
                                    try:
                                        next(ga)
                                    except StopIteration:
                                        live_a = False
                                n += 1
                                if live_b and (n > stagger or not live_a):
                                    try:
                                        next(gb)
                                    except StopIteration:
                                        live_b = False
                        st_ = {0: stage1(0), 1: stage1(1)}
                        for p2 in range(NT // 2):
                            for j in (2 * p2 + 2, 2 * p2 + 3):
                                if j < NT:
                                    st_[j] = stage1(j)
                            run2(stage2(2 * p2, *st_.pop(2 * p2)), stage2(2 * p2 + 1, *st_.pop(2 * p2 + 1)))
                        nbk = sb(phD, "nbk", [128, NE], F32)
                        padded = sb(phD, "padded", [128, NE], F32)
                        pend = sb(phD, "pend", [128, NE], F32)
                        pstart = sb(phD, "pstart", [128, NE], F32)
                        ones32 = sb(phD, "ones32", [128, NE], F32)
                        ps_all = sb(phD, "ps_all", [128, NT * 4], F32)
                        thr = sb(phD, "thr", [128, 1], F32)
                        cmpb = sb(phD, "cmpb", [128, NE], F32)
                        bef = sb(phD, "bef", [128, 1], F32)
                        offc = sb(phD, "offc", [128, 2], F32)
                        offci = sb(phD, "offci", [128, 2], I32)
                        S.do("gpsimd", lambda e: e.memset(ones32[:], 1.0), writes=[ones32])
                        S.do("gpsimd", lambda e: e.iota(thr[:], pattern=[[0, 1]], base=0, channel_multiplier=BLK, allow_small_or_imprecise_dtypes=True), writes=[thr])
                        S.do("vector", lambda e: e.tensor_scalar(out=nbk[:], in0=cnt_bc[:], scalar1=0.0, scalar2=None, op0=ALU.is_gt), reads=[cnt_bc], writes=[nbk])
                        for j in range(1, T // BLK):
                            S.do("vector", lambda e, j=j: e.scalar_tensor_tensor(out=nbk[:], in0=cnt_bc[:], scalar=float(j * BLK), in1=nbk[:], op0=ALU.is_gt, op1=ALU.add),
                                 reads=[cnt_bc], writes=[nbk])
                        S.do("vector", lambda e: e.tensor_scalar(out=padded[:], in0=nbk[:], scalar1=float(BLK), scalar2=None, op0=ALU.mult), reads=[nbk], writes=[padded])
                        S.do("vector", lambda e: e.tensor_tensor_scan(out=pend[:], data0=ones32[:], data1=padded[:], initial=0.0, op0=ALU.mult, op1=ALU.add),
                             reads=[ones32, padded], writes=[pend])
                        S.do("vector", lambda e: e.tensor_tensor(out=pstart[:], in0=pend[:], in1=padded[:], op=ALU.subtract), reads=[pend, padded], writes=[pstart])
                        HB = 32
                        ohs = [(tl, tl.t[:, :].rearrange("p (a b) -> p a b", b=NE)) for tl in t1.tiles]
                        for hh in range(NT * 4 // HB):
                            tl, ov_ = ohs[hh % 2]
                            csl = slice(hh * HB, (hh + 1) * HB)
                            S.do("vector", lambda e, ov_=ov_, csl=csl: e.tensor_tensor(out=ov_, in0=iota32[:].unsqueeze(1).to_broadcast([128, HB, NE]),
                                                                                      in1=e4_all[:, csl].unsqueeze(2).to_broadcast([128, HB, NE]), op=ALU.is_equal),
                                 reads=[iota32, e4a_tok], writes=[tl])
                            S.do("vector", lambda e, ov_=ov_: e.tensor_tensor(out=ov_, in0=ov_, in1=pstart[:].unsqueeze(1).to_broadcast([128, HB, NE]), op=ALU.mult),
                                 reads=[pstart], writes=[tl])
                            S.do("vector", lambda e, ov_=ov_, csl=csl: e.reduce_sum(out=ps_all[:, csl], in_=ov_, axis=AX.X), reads=[tl], writes=[ps_all])
                        S.do("vector", lambda e: e.tensor_tensor(out=ps_all[:], in0=ps_all[:], in1=p4_all[:], op=ALU.add), reads=[p4a_tok], writes=[ps_all])
                        S.do("vector", lambda e: e.tensor_copy(out=gidx_all[:], in_=ps_all[:]), reads=[ps_all], writes=[gidx_tok])
                        be_bc = sb(phD, "be_bc", [128, NBLK], F32)
                        kp = sb(phD, "kp", [128, 8], F32)
                        pidc = sb(phD, "pidc", [128, 1], F32)
                        idxWf = sb(phD, "idxWf", [128, NBLK * 8], F32)
                        idxBf = sb(phD, "idxBf", [128, NBLK], F32)
                        S.do("gpsimd", lambda e: e.iota(kp[:], pattern=[[128, 8]], base=0, channel_multiplier=1, allow_small_or_imprecise_dtypes=True), writes=[kp])
                        S.do("gpsimd", lambda e: e.iota(pidc[:], pattern=[[0, 1]], base=0, channel_multiplier=1, allow_small_or_imprecise_dtypes=True), writes=[pidc])
                        thr48 = sb(phD, "thr48", [128, NBLK], F32)
                        S.do("gpsimd", lambda e: e.iota(thr48[:], pattern=[[BLK, NBLK]], base=0, channel_multiplier=0, allow_small_or_imprecise_dtypes=True), writes=[thr48])
                        for hh, (b0, nb_) in enumerate([(0, HB), (HB, NBLK - HB)]):
                            tl, ov_ = ohs[hh % 2]
                            ovv = ov_[:, 0:nb_, :]
                            S.do("vector", lambda e, ovv=ovv, b0=b0, nb_=nb_: e.tensor_tensor(out=ovv, in0=pend[:].unsqueeze(1).to_broadcast([128, nb_, NE]),
                                                                                             in1=thr48[:, b0:b0 + nb_].unsqueeze(2).to_broadcast([128, nb_, NE]), op=ALU.is_le),
                                 reads=[pend, thr48], writes=[tl])
                            S.do("vector", lambda e, ovv=ovv, b0=b0, nb_=nb_: e.reduce_sum(out=be_bc[:, b0:b0 + nb_], in_=ovv, axis=AX.X), reads=[tl], writes=[be_bc])
                        S.do("vector", lambda e: e.tensor_copy(out=idxB2[:], in_=be_bc[:]), reads=[be_bc], writes=[idx_tok])
                        S.do("vector", lambda e: e.tensor_scalar(out=idxBf[:], in0=be_bc[:], scalar1=128.0, scalar2=pidc[:, 0:1], op0=ALU.mult, op1=ALU.add),
                             reads=[be_bc, pidc], writes=[idxBf])
                        S.do("vector", lambda e: e.tensor_copy(out=idxB[:], in_=idxBf[:]), reads=[idxBf], writes=[idx_tok])
                        S.do("vector", lambda e: e.tensor_scalar(out=be_bc[:], in0=be_bc[:], scalar1=float(D), scalar2=None, op0=ALU.mult), writes=[be_bc])
                        S.do("vector", lambda e: e.tensor_tensor(out=idxWf[:].rearrange("p (b k) -> p b k", k=8), in0=kp[:].unsqueeze(1).to_broadcast([128, NBLK, 8]),
                                                                 in1=be_bc[:].unsqueeze(2).to_broadcast([128, NBLK, 8]), op=ALU.add),
                             reads=[kp, be_bc], writes=[idxWf])
                        S.do("vector", lambda e: e.tensor_copy(out=idxW[:], in_=idxWf[:]), reads=[idxWf], writes=[idx_tok])
                        for i in range(NT):
                            isl = slice(i * 128, (i + 1) * 128)
                            u2b = u2b_r.get()
                            S.dma("sync", lambda e, u2b=u2b, isl=isl: e.dma_start(out=u2b[:], in_=u2s[isl, :]), reads=u2s_tok.tiles, writes=[u2b])
                            for k4 in range(4):
                                col = i * 4 + k4
                                S.dma("gpsimd", lambda e, u2b=u2b, col=col: e.indirect_dma_start(
                                    out=xdisp, out_offset=bass.IndirectOffsetOnAxis(ap=gidx_all[:, col:col + 1], axis=0), in_=u2b[:], in_offset=None,
                                    bounds_check=bnd_reg, oob_is_err=False), reads=[u2b, gidx_tok] + xz_tok.tiles, writes=[sc_tok.get()])
                        S.barrier()
                if stop == "D":
                    S.dead = True

            with ExitStack() as phE:
                pe = Ring([ps(phE, "pe%d" % i, [128, 512], F32) for i in range(8)])
                NRB = CAP // 128
                with ExitStack() as ph:
                    w1g_sb = Ring([sb(ph, "w1g_sb%d" % i, [128, 8, D], BF16) for i in range(2)])
                    w1u_sb = Ring([sb(ph, "w1u_sb%d" % i, [128, 8, D], BF16) for i in range(2)])
                    w2_sb = Ring([sb(ph, "w2_sb%d" % i, [128, 8, D], BF16) for i in range(2)])
                    xe_tm = Ring([sb(ph, "xe_tm%d" % i, [128, NRB, D], BF16) for i in range(2)])

                    def load_xt(bi):
                        xt_ = xe_tm.get()
                        S.dma("sync", lambda e, xt_=xt_, bi=bi: e.dma_start(
                            out=xt_[:], in_=xdisp[bi * CAP:(bi + 1) * CAP, :].rearrange("(r p) d -> p r d", p=128)),
                            reads=sc_tok.tiles + xz_tok.tiles, writes=[xt_])
                        return xt_
                    xt_next = load_xt(0)
                    xeT = Ring([sb(ph, "xeT%d" % i, [128, 8, CAP], BF16) for i in range(2)])
                    aTt = Ring([sb(ph, "aTt%d" % i, [128, 8, CAP], BF16) for i in range(2)])
                    s_r = Ring([sb(ph, "s_r%d" % i, [128, CAP], F32) for i in range(3)])
                    t_r = Ring([sb(ph, "t_r%d" % i, [128, CAP], F32) for i in range(3)])
                    ysb = Ring([sb(ph, "ysb%d" % i, [128, D], F32) for i in range(2)])
                    b2bc = Ring([sb(ph, "b2bc%d" % i, [128, D], F32) for i in range(2)])
                    bgt_r = Ring([sb(ph, "bgt%d" % i, [128, 8], F32) for i in range(2)])
                    but_r = Ring([sb(ph, "but%d" % i, [128, 8], F32) for i in range(2)])
                    WAP = [[D, 128], [128 * D, 8], [1, D]]
                    for ex_ in range(NBLK):
                        wgs, wus, w2s = w1g_sb.get(), w1u_sb.get(), w2_sb.get()
                        xT, bb, bgt, but = xeT.get(), b2bc.get(), bgt_r.get(), but_r.get()
                        xt = xt_next
                        if ex_ + 1 < NBLK:
                            xt_next = load_xt(ex_ + 1)
                        def gath(dst, tab, idxap, breg_):
                            return lambda e: e.indirect_dma_start(out=dst, out_offset=None, in_=tab,
                                                                  in_offset=bass.IndirectOffsetOnAxis(ap=idxap, axis=0),
                                                                  bounds_check=breg_, oob_is_err=False)
                        S.dma("gpsimd", [gath(wgs[:, k, :], w1g2, idxW[:, ex_ * 8 + k:ex_ * 8 + k + 1], bw_reg) for k in range(8)],
                              reads=[idx_tok], writes=[wgs])
                        S.dma("gpsimd", gath(bgt[:], b1gE2, idxB[:, ex_:ex_ + 1], bb_reg), reads=[idx_tok], writes=[bgt])
                        S.dma("gpsimd", gath(but[:], b1uE2, idxB[:, ex_:ex_ + 1], bb_reg), reads=[idx_tok], writes=[but])
                        S.dma("gpsimd", [gath(wus[:, k, :], w1u2, idxW[:, ex_ * 8 + k:ex_ * 8 + k + 1], bw_reg) for k in range(8)],
                              reads=[idx_tok], writes=[wus])
                        S.dma("gpsimd", [gath(w2s[:, k, :], w22, idxW[:, ex_ * 8 + k:ex_ * 8 + k + 1], bw_reg) for k in range(8)],
                              reads=[idx_tok], writes=[w2s])
                        S.dma("gpsimd", gath(bb[:], b2v, idxB2[:, ex_:ex_ + 1], b2_reg), reads=[idx_tok], writes=[bb])
                        S.do("vector", lambda e, bgt=bgt: e.tensor_scalar(out=bgt[:], in0=bgt[:], scalar1=1.702, scalar2=None, op0=ALU.mult), writes=[bgt])
                        S.do("vector", lambda e, but=but: e.tensor_scalar_add(out=but[:], in0=but[:], scalar1=1.0), writes=[but])
                        for rb in range(NRB):
                            pt = pe.get()
                            ptv = pt.t.bitcast(BF16)

                            def trf(e, ptv=ptv, xt=xt, rb=rb):
                                ins = None
                                for k in range(8):
                                    ins = e.transpose(out=ptv[:, k * 128:(k + 1) * 128], in_=xt[:, rb, k * 128:(k + 1) * 128], identity=ident_bf[:])
                                return ins
                            S.do("tensor", trf, reads=[xt, ident_bf], writes=[pt])
                            S.do("scalar", lambda e, ptv=ptv, xT=xT, rb=rb: e.copy(out=xT[:, :, rb * 128:(rb + 1) * 128],
                                                                              in_=ptv[:, :].rearrange("p (k m) -> p k m", k=8)),
                                 reads=[pt], writes=[xT])
                        at = aTt.get()
                        for fc in range(8):
                            bg = pe.get()
                            mm(bg, bg[:, 0:CAP], [(wgs[:, k, fc * 128:(fc + 1) * 128], xT[:, k, :]) for k in range(8)], [wgs, xT])
                            bu = pe.get()
                            mm(bu, bu[:, 0:CAP], [(wus[:, k, fc * 128:(fc + 1) * 128], xT[:, k, :]) for k in range(8)], [wus, xT])
                            s_, t_ = s_r.get(), t_r.get()
                            S.do("scalar", lambda e, bg=bg, s_=s_, fc=fc, bgt=bgt: e.activation(out=s_[:], in_=bg[:, 0:CAP], func=AF.Silu,
                                                                                           bias=bgt[:, fc:fc + 1], scale=1.702),
                                 reads=[bg, bgt], writes=[s_])
                            S.do("scalar", lambda e, bu=bu, t_=t_, fc=fc, but=but: e.activation(out=t_[:], in_=bu[:, 0:CAP], func=AF.Identity,
                                                                                           bias=but[:, fc:fc + 1], scale=1.0),
                                 reads=[bu, but], writes=[t_])
                            S.do("vector", lambda e, t_=t_: e.tensor_scalar(out=t_[:], in0=t_[:], scalar1=-6.0, scalar2=8.0, op0=ALU.max, op1=ALU.min),
                                 writes=[t_])
                            S.do("vector", lambda e, s_=s_, t_=t_, at=at, fc=fc: e.scalar_tensor_tensor(out=at[:, fc, :], in0=s_[:], scalar=C7, in1=t_[:],
                                                                                                      op0=ALU.min, op1=ALU.mult),
                                 reads=[s_, t_], writes=[at])
                        for rb in range(NRB):
                            ys = ysb.get()
                            for n in range(2):
                                nsl = slice(n * 512, (n + 1) * 512)
                                by = pe.get()
                                mm(by, by[:], [(at[:, fc, rb * 128:(rb + 1) * 128], w2s[:, fc, nsl]) for fc in range(8)], [w2s, at])
                                S.do("vector", lambda e, by=by, ys=ys, bb=bb, nsl=nsl: e.scalar_tensor_tensor(
                                    out=ys[:, nsl], in0=by[:], scalar=1.0 / 1.702, in1=bb[:, nsl], op0=ALU.mult, op1=ALU.add),
                                    reads=[by, bb], writes=[ys])
                            r0 = ex_ * CAP + rb * 128
                            S.dma("sync", lambda e, ys=ys, r0=r0: e.dma_start(out=ydisp[r0:r0 + 128, :], in_=ys[:]), reads=[ys], writes=[yw_tok.get()])
                    S.barrier()
                with ExitStack() as ph:
                    g2_bc = sb(ph, "g2_bc", [128, D], F32)
                    b2_bc = sb(ph, "b2_bc", [128, D], F32)
                    x1r = Ring([sb(ph, "x1r%d" % i, [128, D], F32) for i in range(3)])
                    yg = Ring([sb(ph, "yg%d" % i, [128, D], F32) for i in range(12)])
                    ysum = Ring([sb(ph, "ysum%d" % i, [128, D], F32) for i in range(2)])
                    zt = Ring([sb(ph, "ztE%d" % i, [128, D], F32) for i in range(2)])
                    ot_ = Ring([sb(ph, "otE%d" % i, [128, D], F32) for i in range(2)])
                    stats = Ring([sb(ph, "statsE%d" % i, [128, 2, 6], F32) for i in range(2)])
                    mv = Ring([sb(ph, "mvE%d" % i, [128, 2], F32) for i in range(2)])
                    rsd = Ring([sb(ph, "rsdE%d" % i, [128, 1], F32) for i in range(2)])
                    out_tok = Ring([Tok() for _ in range(4)])
                    S.dma("sync", lambda e: e.dma_start(out=g2_bc[:], in_=ln2g.partition_broadcast(128)), writes=[g2_bc])
                    S.dma("sync", lambda e: e.dma_start(out=b2_bc[:], in_=ln2b.partition_broadcast(128)), writes=[b2_bc])
                    def prefetch(i):
                        isl = slice(i * 128, (i + 1) * 128)
                        xr = x1r.get()
                        S.dma("sync", lambda e, xr=xr, isl=isl: e.dma_start(out=xr[:], in_=x1s[isl, :]), reads=[x1s_tok[i]], writes=[xr])
                        gs = []
                        for k4 in range(4):
                            g_ = yg.get()
                            col = i * 4 + k4
                            S.dma("gpsimd", lambda e, g_=g_, col=col: e.indirect_dma_start(
                                out=g_[:], out_offset=None, in_=ydisp, in_offset=bass.IndirectOffsetOnAxis(ap=gidx_all[:, col:col + 1], axis=0),
                                bounds_check=bnd_reg, oob_is_err=False), reads=[gidx_tok] + yw_tok.tiles, writes=[g_])
                            gs.append(g_)
                        return xr, gs
                    pre = [prefetch(0), prefetch(1)]
                    for i in range(NT):
                        isl = slice(i * 128, (i + 1) * 128)
                        if i + 2 < NT:
                            pre.append(prefetch(i + 2))
                        xr, gs = pre[i]
                        ysm, zz, oo = ysum.get(), zt.get(), ot_.get()
                        for k4 in range(4):
                            g_ = gs[k4]
                            col = i * 4 + k4
                            if k4 == 0:
                                S.do("scalar", lambda e, g_=g_, ysm=ysm, col=col: e.activation(out=ysm[:], in_=g_[:], func=AF.Identity,
                                                                                            scale=w4_all[:, col:col + 1]),
                                     reads=[g_, w4_tok], writes=[ysm])
                            else:
                                S.do("vector", lambda e, g_=g_, ysm=ysm, col=col: e.scalar_tensor_tensor(out=ysm[:], in0=g_[:], scalar=w4_all[:, col:col + 1],
                                                                                                     in1=ysm[:], op0=ALU.mult, op1=ALU.add),
                                     reads=[g_, w4_tok], writes=[ysm])
                        S.do("vector", lambda e, ysm=ysm: e.tensor_tensor(out=ysm[:], in0=ysm[:], in1=gf_bc[:], op=ALU.mult), reads=[gf_bc], writes=[ysm])
                        S.do("vector", lambda e, xr=xr, ysm=ysm, zz=zz: e.scalar_tensor_tensor(out=zz[:], in0=xr[:], scalar=ALPHA, in1=ysm[:],
                                                                                              op0=ALU.mult, op1=ALU.add),
                             reads=[xr, ysm], writes=[zz])
                        layer_norm(zz, oo, g2_bc, b2_bc, stats.get(), mv.get(), rsd.get(), eng_gb="vector", act_norm=True)
                        S.dma("sync", lambda e, oo=oo, isl=isl: e.dma_start(out=out[isl, :], in_=oo[:]), reads=[oo], writes=[out_tok.get()])
                    S.barrier()
        except _Stop:
            pass
        S.barrier(force=True)
        with nc.Block() as block:
            S.emit(block)
    return nc


_NC_CACHE = {}


def _prep_inputs(inp, b):
    f = lambda a: np.ascontiguousarray(np.asarray(a, dtype=np.float32))
    col = lambda v: f(np.asarray(v).reshape(-1, 128).T)
    wgu = np.asarray(inp["w_gate_up"])[0]
    bgu = np.asarray(inp["b_gate_up"])[0]
    m = {
        "x": f(inp["x"][b]),
        "cT": col(inp["c"][b]),
        "w_ada": f(inp["w_ada"][0]),
        "b_ada": f(inp["b_ada"][0]).reshape(1, -1),
        "w_in": f(inp["w_in"][0]),
        "wpg": f(inp["w_pool_group"][0]),
        "pscT": col(inp["pool_scale"][0]),
        "wba": f(inp["w_branch_a"][0]),
        "wau": f(inp["w_alpha_up"][0]),
        "baT": col(inp["b_alpha"][0]),
        "gainT": col(inp["gla_norm_gain"][0]),
        "wbb": f(inp["w_branch_b"][0]),
        "wout": f(inp["w_out"][0]),
        "ln1g": f(inp["ln1_gain"][0]).reshape(1, -1),
        "ln1b": f(inp["ln1_bias"][0]).reshape(1, -1),
        "wr": f(inp["w_router"][0]),
        "br": f(inp["b_router"][0]).reshape(1, -1),
        "w1g": f(wgu[:, :, 0::2]),
        "w1u": f(wgu[:, :, 1::2]),
        "b1gE": f(bgu[:, 0::2].reshape(NE, 8, 128).transpose(0, 2, 1)),
        "b1uE": f(bgu[:, 1::2].reshape(NE, 8, 128).transpose(0, 2, 1)),
        "w2": f(inp["w_down"][0]),
        "b2": f(inp["b_down"][0]),
        "ln2g": f(inp["ln2_gain"][0]).reshape(1, -1),
        "ln2b": f(inp["ln2_bias"][0]).reshape(1, -1),
    }
    return m


def kernel(**inputs):
    inputs = {k: np.asarray(v) for k, v in inputs.items()}
    if "nc" not in _NC_CACHE:
        _NC_CACHE["nc"] = build()
    nc = _NC_CACHE["nc"]
    shared = _prep_inputs(inputs, 0)
    in_maps = []
    for b in range(8):
        m = dict(shared)
        m["x"] = np.ascontiguousarray(inputs["x"][b], dtype=np.float32)
        m["cT"] = np.ascontiguousarray(np.asarray(inputs["c"][b], dtype=np.float32).reshape(-1, 128).T)
        in_maps.append(m)
    res = run_bass_kernel_spmd(nc, in_maps, core_ids=list(range(8)))
    return np.stack([np.asarray(r["out"], dtype=np.float32) for r in res.results], axis=0)
```

```python
import numpy as np
from contextlib import ExitStack
import concourse.bass as bass
import concourse.mybir as mybir
from concourse.bass_utils import run_bass_kernel_spmd

F32 = mybir.dt.float32
BF16 = mybir.dt.bfloat16
AF = mybir.ActivationFunctionType
ALU = mybir.AluOpType
AX = mybir.AxisListType

ENGS = ["tensor", "vector", "scalar", "gpsimd", "sync"]
T = 2048
NT = 16
D = 1024
KC = 8
NE = 32
IN_TOTAL = 6160
BLK = 384
CAP = BLK
NBLK = (T * 4 + NE * (BLK - 1) + BLK - 1) // BLK
NROWS = NBLK * BLK
I32 = mybir.dt.int32
U32 = mybir.dt.uint32
ALPHA = 2.0 ** 0.25
C7 = 1.702 * 7.0 / (1.0 + float(np.exp(-1.702 * 7.0)))


class Tok:
    __slots__ = ("w", "r", "sem")

    def __init__(self):
        self.w = {}
        self.r = {}
        self.sem = None


class Tile(Tok):
    __slots__ = ("t",)

    def __init__(self, t):
        Tok.__init__(self)
        self.t = t

    def __getitem__(self, k):
        return self.t[k]


class Sched:
    def __init__(self, nc, stack):
        self.nc = nc
        self.stack = stack
        self.q = {e: [] for e in ENGS}
        self.sems = {}
        self.count = {}
        self.seen = {e: {} for e in ENGS}
        for e in ENGS:
            self.sems[e] = stack.enter_context(nc.semaphore("s_" + e))
            self.count[e] = 0
        self.ndma = 0
        self.dead = False
        self.free_sems = []
        self.sem_order = []
        self.max_dma_sems = 100
        self.rr = 0
        self.last_mark = 0

    def mark(self):
        return len(self.sem_order)

    def recycle(self, mark):
        for sm in self.sem_order[mark:]:
            if sm not in self.free_sems:
                self.free_sems.append(sm)
        del self.sem_order[mark:]

    def _waits(self, eng, need):
        out = []
        for k, v in need.items():
            if v > self.seen[eng].get(k, 0):
                self.seen[eng][k] = v
                out.append((k, v))
        return out

    @staticmethod
    def _merge(need, d):
        for k, v in d.items():
            if v > need.get(k, 0):
                need[k] = v

    def _deps(self, reads, writes):
        need = {}
        for t in reads:
            self._merge(need, t.w)
        for t in writes:
            self._merge(need, t.w)
            self._merge(need, t.r)
        return need

    def _commit(self, tk, reads, writes):
        k, v = tk
        for t in reads:
            if v > t.r.get(k, 0):
                t.r[k] = v
        for t in writes:
            t.w = {k: v}
            t.r = {}

    def do(self, eng, fn, reads=(), writes=()):
        if self.dead:
            return (eng, 0)
        need = self._deps(reads, writes)
        w = self._waits(eng, need)
        self.count[eng] += 1
        tk = (eng, self.count[eng])
        self.q[eng].append((w, fn, (eng, 1)))
        self._commit(tk, reads, writes)
        return tk

    def dma(self, eng, fn, reads=(), writes=()):
        if self.dead:
            return (eng, 0)
        tok = writes[0]
        if tok.sem is None:
            fl = [x for x in self.free_sems if x.startswith(eng[0])]
            if fl:
                tok.sem = fl[0]
                self.free_sems.remove(fl[0])
            else:
                self.ndma += 1
                tok.sem = "%s%d" % (eng[0], self.ndma)
                self.sems[tok.sem] = self.stack.enter_context(self.nc.semaphore("s_" + tok.sem))
                self.count[tok.sem] = 0
            self.sem_order.append(tok.sem)
        sem = tok.sem
        need = self._deps(reads, writes)
        if self.count[sem] > need.get(sem, 0):
            need[sem] = self.count[sem]
        w = self._waits(eng, need)
        fns = fn if isinstance(fn, (list, tuple)) else [fn]
        for j, f1 in enumerate(fns):
            self.count[sem] += 16
            self.q[eng].append((w if j == 0 else [], f1, (sem, 16)))
        tk = (sem, self.count[sem])
        self._commit(tk, reads, writes)
        return tk

    def raw(self, eng, fn, reads=()):
        if self.dead:
            return
        need = self._deps(reads, ())
        self.q[eng].append((self._waits(eng, need), fn, None))

    def barrier(self, force=False):
        if self.dead and not force:
            return
        need = dict(self.count)
        for e in ENGS:
            self.q[e].append((self._waits(e, dict(need)), None, None))
        self.recycle(self.last_mark)
        self.last_mark = self.mark()

    def emit(self, block):
        for e_name in ENGS:
            q = self.q[e_name]

            def body(engine, q=q):
                for (w, fn, inc) in q:
                    for (k, v) in w:
                        engine.wait_ge(self.sems[k], v)
                    if fn is None:
                        continue
                    ins = fn(engine)
                    if inc is not None:
                        ins.then_inc(self.sems[inc[0]], inc[1])

            getattr(block, e_name)(body)


class _Stop(Exception):
    pass


class Ring:
    def __init__(self, tiles):
        self.tiles = tiles
        self.i = 0

    def get(self):
        t = self.tiles[self.i % len(self.tiles)]
        self.i += 1
        return t


def build(dbg=False, stop=None):
    nc = bass.Bass("TRN2", target_bir_lowering=False)

    def din(name, shape):
        return nc.dram_tensor(name, shape, F32, kind="ExternalInput").ap()

    x = din("x", [T, D])
    cT = din("cT", [128, 8])
    w_ada = din("w_ada", [D, 6 * D])
    b_ada = din("b_ada", [1, 6 * D])
    w_in = din("w_in", [D, IN_TOTAL])
    wpg = din("wpg", [4, 256, 256])
    pscT = din("pscT", [128, 8])
    wba = din("wba", [D, D])
    wau = din("wau", [16, 512])
    baT = din("baT", [128, 4])
    gainT = din("gainT", [128, 2])
    wbb = din("wbb", [D, D])
    wout = din("wout", [D, D])
    ln1g = din("ln1g", [1, D])
    ln1b = din("ln1b", [1, D])
    wr = din("wr", [D, NE])
    br = din("br", [1, NE])
    w1g_h = nc.dram_tensor("w1g", [NE, D, D], F32, kind="ExternalInput")
    w1u_h = nc.dram_tensor("w1u", [NE, D, D], F32, kind="ExternalInput")
    b1gE_h = nc.dram_tensor("b1gE", [NE, 128, 8], F32, kind="ExternalInput")
    b1uE_h = nc.dram_tensor("b1uE", [NE, 128, 8], F32, kind="ExternalInput")
    w2_h = nc.dram_tensor("w2", [NE, D, D], F32, kind="ExternalInput")
    b2_h = nc.dram_tensor("b2", [NE, D], F32, kind="ExternalInput")
    w1g2 = w1g_h.ap().rearrange("e r n -> (e r) n")
    w1u2 = w1u_h.ap().rearrange("e r n -> (e r) n")
    w22 = w2_h.ap().rearrange("e r n -> (e r) n")
    b1gE2 = b1gE_h.ap().rearrange("e p f -> (e p) f")
    b1uE2 = b1uE_h.ap().rearrange("e p f -> (e p) f")
    b2v = b2_h.ap()
    ln2g = din("ln2g", [1, D])
    ln2b = din("ln2b", [1, D])
    out = nc.dram_tensor("out", [T, D], F32, kind="ExternalOutput").ap()
    x1s = nc.dram_tensor("x1s", [T, D], F32).ap()
    xdisp = nc.dram_tensor("xdisp", [NROWS, D], BF16).ap()
    ydisp = nc.dram_tensor("ydisp", [NROWS, D], F32).ap()
    u2s = nc.dram_tensor("u2s", [T, D], BF16).ap()
    dbg_out = {}
    if dbg:
        for nm, shp in [("d_mod", [128, 6 * D]), ("d_u1T", [128, 8 * T]), ("d_obT", [128, 8 * T]), ("d_ya1T", [128, 8 * T]),
                        ("d_mT", [128, 8 * T]), ("d_x1", [T, D])]:
            dbg_out[nm] = nc.dram_tensor(nm, shp, F32, kind="ExternalOutput").ap()

    w_in_v = w_in.rearrange("(k p) n -> p k n", p=128)
    w_ada_v = w_ada.rearrange("(k p) n -> p k n", p=128)
    wba_v = wba.rearrange("(k p) n -> p k n", p=128)
    wbb_v = wbb.rearrange("(k p) n -> p k n", p=128)
    wout_v = wout.rearrange("(k p) n -> p k n", p=128)
    wr_v = wr.rearrange("(k p) n -> p k n", p=128)

    with ExitStack() as st0:
        S = Sched(nc, st0)
        bnd_reg = st0.enter_context(nc.gpsimd.register("bnd_reg"))
        bw_reg = st0.enter_context(nc.gpsimd.register("bw_reg"))
        bb_reg = st0.enter_context(nc.gpsimd.register("bb_reg"))
        b2_reg = st0.enter_context(nc.gpsimd.register("b2_reg"))
        S.q["gpsimd"].append(([], lambda e: e.reg_mov(bw_reg, NE * D - 1), None))
        S.q["gpsimd"].append(([], lambda e: e.reg_mov(bb_reg, NE * 128 - 1), None))
        S.q["gpsimd"].append(([], lambda e: e.reg_mov(b2_reg, NE - 1), None))
        S.q["gpsimd"].append(([], lambda e: e.reg_mov(bnd_reg, NROWS - 1), None))
        try:

            uniq = [0]

            def sb(stack, name, shape, dt):
                uniq[0] += 1
                return Tile(stack.enter_context(nc.sbuf_tensor("%s_%d" % (name, uniq[0]), shape, dt)))

            def ps(stack, name, shape, dt):
                uniq[0] += 1
                return Tile(stack.enter_context(nc.psum_tensor("%s_%d" % (name, uniq[0]), shape, dt)))

            def mm(outt, out_ap, pairs, reads):
                def fn(e):
                    ins = None
                    n = len(pairs)
                    for i, (l, r) in enumerate(pairs):
                        ins = e.matmul(out_ap, lhsT=l, rhs=r, start=(i == 0), stop=(i == n - 1))
                    return ins
                return S.do("tensor", fn, reads=reads, writes=[outt])

            def dbg_dump(name, tile, ap, tmpdt=None):
                if not dbg:
                    return
                S.barrier()
                if tmpdt is None:
                    S.dma("sync", lambda e: e.dma_start(out=dbg_out[name], in_=ap), reads=[tile], writes=[Tok()])
                else:
                    S.dma("gpsimd", lambda e: e.dma_start(out=dbg_out[name], in_=ap), reads=[tile], writes=[Tok()])
                S.barrier()

            ident_bf = sb(st0, "ident_bf", [128, 128], BF16)
            ident_f = sb(st0, "ident_f", [128, 128], F32)
            ones_bf = sb(st0, "ones_bf", [128, 128], BF16)
            cmask4 = sb(st0, "cmask4", [128, 512], F32)
            mask01 = sb(st0, "mask01", [128, 512], F32)
            rc16 = sb(st0, "rc16", [128, 16], F32)
            gf_bc = sb(st0, "gf_bc", [128, D], F32)
            uT_tok = [Tok() for _ in range(NT)]
            gidx_all = sb(st0, "gidx_all", [128, NT * 4], I32)
            w4_all = sb(st0, "w4_all", [128, NT * 4], F32)
            e4_all = sb(st0, "e4_all", [128, NT * 4], F32)
            p4_all = sb(st0, "p4_all", [128, NT * 4], F32)
            e4a_tok, p4a_tok = Tok(), Tok()
            idxW = sb(st0, "idxW", [128, NBLK * 8], I32)
            idxB = sb(st0, "idxB", [128, NBLK], I32)
            idxB2 = sb(st0, "idxB2", [128, NBLK], I32)
            idx_tok = Tok()
            u2s_tok = Ring([Tok() for _ in range(4)])
            iota32 = sb(st0, "iota32", [128, NE], F32)
            Ltri = sb(st0, "Ltri", [128, 128], BF16)
            xz_tok = Ring([Tok() for _ in range(4)])
            sc_tok = Ring([Tok() for _ in range(8)])
            yw_tok = Ring([Tok() for _ in range(8)])

            def uT_reads(tt):
                return uT_tok[tt * 4:(tt + 1) * 4]

            S.do("gpsimd", lambda e: e.memset(ones_bf[:], 1.0), writes=[ones_bf])
            S.do("gpsimd", lambda e: e.affine_select(out=ident_bf[:], in_=ones_bf[:], pattern=[[-1, 128]],
                                                     compare_op=ALU.is_equal, fill=0.0, base=0, channel_multiplier=1),
                 reads=[ones_bf], writes=[ident_bf])
            S.do("vector", lambda e: e.tensor_copy(out=ident_f[:], in_=ident_bf[:]), reads=[ident_bf], writes=[ident_f])
            S.do("gpsimd", lambda e: e.memset(cmask4[:], 1.0), writes=[cmask4])
            S.do("gpsimd", lambda e: e.affine_select(out=cmask4[:], in_=cmask4[:], pattern=[[0, 4], [1, 128]],
                                                     compare_op=ALU.is_ge, fill=0.0, base=0, channel_multiplier=-1),
                 reads=[cmask4], writes=[cmask4])
            S.do("gpsimd", lambda e: e.memset(mask01[:], 1.0), writes=[mask01])
            S.do("gpsimd", lambda e: e.memset(mask01[:].rearrange("p (c j) -> p c j", j=128)[:, :, 0:1], 0.0),
                 writes=[mask01])
            S.do("gpsimd", lambda e: e.iota(rc16[:], pattern=[[1, 16]], base=1, channel_multiplier=0,
                                            allow_small_or_imprecise_dtypes=True), writes=[rc16])
            S.do("vector", lambda e: e.reciprocal(out=rc16[:], in_=rc16[:]), reads=[rc16], writes=[rc16])
            S.do("gpsimd", lambda e: e.iota(iota32[:], pattern=[[1, NE]], base=0, channel_multiplier=0,
                                            allow_small_or_imprecise_dtypes=True), writes=[iota32])
            S.do("gpsimd", lambda e: e.memset(Ltri[:], 1.0), writes=[Ltri])
            S.do("gpsimd", lambda e: e.affine_select(out=Ltri[:], in_=Ltri[:], pattern=[[1, 128]], compare_op=ALU.is_gt,
                                                     fill=0.0, base=0, channel_multiplier=-1), writes=[Ltri])

            with ExitStack() as stM:
                uT = sb(stM, "uT", [128, 8, T], BF16)
                mod_bc = sb(stM, "mod_bc", [128, 6 * D], F32)
                SH_M, SC_M, G_M, SH_F, SC_F, G_F = [slice(i * D, (i + 1) * D) for i in range(6)]

                with ExitStack() as ph:
                    c_sb = sb(ph, "c_sb", [128, 8], F32)
                    sc = sb(ph, "sc", [128, 8], F32)
                    scb = sb(ph, "scb", [128, 8, 128], BF16)
                    wa = Ring([sb(ph, "wa%d" % i, [128, 8, 512], BF16) for i in range(2)])
                    psA = Ring([ps(ph, "psA%d" % i, [128, 512], F32) for i in range(2)])
                    S.dma("sync", lambda e: e.dma_start(out=c_sb[:], in_=cT), writes=[c_sb])
                    S.dma("sync", lambda e: e.dma_start(out=mod_bc[:], in_=b_ada.partition_broadcast(128)), writes=[mod_bc])
                    S.do("scalar", lambda e: e.activation(out=sc[:], in_=c_sb[:], func=AF.Silu), reads=[c_sb], writes=[sc])
                    for k in range(8):
                        S.do("vector", lambda e, k=k: e.tensor_scalar(out=scb[:, k, :], in0=ones_bf[:], scalar1=sc[:, k:k + 1],
                                                                      scalar2=None, op0=ALU.mult),
                             reads=[ones_bf, sc], writes=[scb])
                    for j in range(12):
                        wt = wa.get()
                        S.dma("gpsimd", lambda e, wt=wt, j=j: e.dma_start(out=wt[:], in_=w_ada_v[:, :, j * 512:(j + 1) * 512]),
                              writes=[wt])
                        pt = psA.get()
                        mm(pt, pt[:], [(scb[:, k, :], wt[:, k, :]) for k in range(8)], [scb, wt])
                        S.do("vector", lambda e, pt=pt, j=j: e.tensor_tensor(out=mod_bc[:, j * 512:(j + 1) * 512], in0=pt[:],
                                                                             in1=mod_bc[:, j * 512:(j + 1) * 512], op=ALU.add),
                             reads=[pt], writes=[mod_bc])
                    S.do("vector", lambda e: e.tensor_scalar_add(out=mod_bc[:, SC_M], in0=mod_bc[:, SC_M], scalar1=1.0),
                         writes=[mod_bc])
                    S.do("vector", lambda e: e.tensor_scalar_add(out=mod_bc[:, SC_F], in0=mod_bc[:, SC_F], scalar1=1.0),
                         writes=[mod_bc])
                    S.do("vector", lambda e: e.tensor_copy(out=gf_bc[:], in_=mod_bc[:, G_F]), reads=[mod_bc], writes=[gf_bc])
                    dbg_dump("d_mod", mod_bc, mod_bc[:])
                    S.barrier()
                    if stop == "A":
                        S.dead = True

                with ExitStack() as ph:
                    xin = Ring([sb(ph, "xin%d" % i, [128, D], F32) for i in range(4)])
                    tmpf = Ring([sb(ph, "tmpf%d" % i, [128, D], F32) for i in range(3)])
                    u1b = Ring([sb(ph, "u1b%d" % i, [128, D], BF16) for i in range(3)])
                    ptr = Ring([ps(ph, "ptr%d" % i, [128, 8, 128], BF16) for i in range(2)])
                    for i in range(NT):
                        xi, tf, ub, pt = xin.get(), tmpf.get(), u1b.get(), ptr.get()
                        S.dma("sync", lambda e, xi=xi, i=i: e.dma_start(out=xi[:], in_=x[i * 128:(i + 1) * 128, :]), writes=[xi])
                        S.do("vector", lambda e, xi=xi, tf=tf: e.tensor_tensor(out=tf[:], in0=xi[:], in1=mod_bc[:, SC_M], op=ALU.mult),
                             reads=[xi, mod_bc], writes=[tf])
                        S.do("gpsimd", lambda e, tf=tf, ub=ub: e.tensor_tensor(out=ub[:], in0=tf[:], in1=mod_bc[:, SH_M], op=ALU.add),
                             reads=[tf, mod_bc], writes=[ub])

                        def trf(e, ub=ub, pt=pt):
                            ins = None
                            for k in range(8):
                                ins = e.transpose(out=pt[:, k, :], in_=ub[:, k * 128:(k + 1) * 128], identity=ident_bf[:])
                            return ins
                        S.do("tensor", trf, reads=[ub, ident_bf], writes=[pt])
                        S.do("scalar", lambda e, pt=pt, i=i: e.copy(out=uT[:, :, i * 128:(i + 1) * 128], in_=pt[:]),
                             reads=[pt], writes=[uT_tok[i]])
                    dbg_dump("d_u1T", uT, uT[:].rearrange("p k t -> p (k t)"), BF16)
                    S.barrier()
                    if stop == "B":
                        S.dead = True

                with ExitStack() as phC:
                    obT = sb(phC, "obT", [128, 8, T], BF16)
                    obT_tok = [[Tok() for _ in range(4)] for _ in range(8)]
                    ya1T_tok = [Tok() for _ in range(8)]
                    pb = Ring([ps(phC, "pb%d" % i, [128, 512], F32) for i in range(7)])
                    ptb = ps(phC, "ptb", [128, 4, 128], BF16)

                    with ExitStack() as ph:
                        wal = sb(ph, "wal", [128, 8, 16], BF16)
                        alT = sb(ph, "alT", [16, T], BF16)
                        wau_sb = sb(ph, "wau_sb", [16, 512], BF16)
                        nba = sb(ph, "nba", [128, 4], F32)
                        gain_sb = sb(ph, "gain_sb", [128, 2], F32)
                        whd = Ring([sb(ph, "whd%d" % i, [128, 8, 768], BF16) for i in range(2)])
                        tA = Ring([sb(ph, "tA%d" % i, [128, 512], F32) for i in range(2)])
                        tB = Ring([sb(ph, "tB%d" % i, [128, 512], F32) for i in range(2)])
                        ebt = Ring([sb(ph, "ebt%d" % i, [128, 512], F32) for i in range(2)])
                        enbt = Ring([sb(ph, "enbt%d" % i, [128, 512], F32) for i in range(2)])
                        qsT = Ring([sb(ph, "qsT%d" % i, [128, 512], BF16) for i in range(2)])
                        ksT = Ring([sb(ph, "ksT%d" % i, [128, 512], BF16) for i in range(2)])
                        v_sb = Ring([sb(ph, "v_sb%d" % i, [128, 4, 256], BF16) for i in range(2)])
                        kt_sb = Ring([sb(ph, "kt_sb%d" % i, [128, 4, 128], BF16) for i in range(2)])
                        sT_sb = Ring([sb(ph, "sT_sb%d" % i, [128, 512], BF16) for i in range(2)])
                        Ttmp = Ring([sb(ph, "Ttmp%d" % i, [128, 256], F32) for i in range(2)])
                        Sf = sb(ph, "Sf", [128, 256], F32)
                        Sb = Ring([sb(ph, "Sb%d" % i, [128, 256], BF16) for i in range(2)])
                        oT_sb = Ring([sb(ph, "oT_sb%d" % i, [128, 2, 512], F32) for i in range(1)])
                        sq = Ring([sb(ph, "sq%d" % i, [128, 2, 512], BF16) for i in range(1)])
                        rstd = Ring([sb(ph, "rstd%d" % i, [128, 512], F32) for i in range(2)])
                        srt = Ring([sb(ph, "srt%d" % i, [128, 512], F32) for i in range(2)])
                        zb = Ring([sb(ph, "zb%d" % i, [128, 512], F32) for i in range(2)])

                        zt4 = sb(ph, "zt4", [128, 2 * D], BF16)
                        S.do("gpsimd", lambda e: e.memset(zt4[:], 0.0), writes=[zt4])
                        for zi in range(NROWS // 256):
                            S.dma("sync", lambda e, zi=zi: e.dma_start(out=xdisp[zi * 256:(zi + 1) * 256, :].rearrange("(p a) d -> p (a d)", p=128),
                                                                      in_=zt4[:]), reads=[zt4], writes=[xz_tok.get()])
                        S.dma("gpsimd", lambda e: e.dma_start(out=wal[:], in_=w_in_v[:, :, 4096:4112]), writes=[wal])
                        S.dma("gpsimd", lambda e: e.dma_start(out=wau_sb[:], in_=wau), writes=[wau_sb])
                        S.dma("sync", lambda e: e.dma_start(out=nba[:], in_=baT), writes=[nba])
                        S.dma("sync", lambda e: e.dma_start(out=gain_sb[:], in_=gainT), writes=[gain_sb])
                        S.do("vector", lambda e: e.tensor_scalar(out=nba[:], in0=nba[:], scalar1=-1.0, scalar2=None, op0=ALU.mult),
                             writes=[nba])
                        for tt in range(4):
                            pt = pb.get()
                            mm(pt, pt[0:16, :], [(wal[:, k, :], uT[:, k, tt * 512:(tt + 1) * 512]) for k in range(8)],
                               [wal] + uT_reads(tt))
                            S.do("scalar", lambda e, pt=pt, tt=tt: e.copy(out=alT[:, tt * 512:(tt + 1) * 512], in_=pt[0:16, :]),
                                 reads=[pt], writes=[alT])

                        if stop == "C1":
                            S.dead = True
                        def load_head(h):
                            wh_ = whd.get()
                            S.dma("gpsimd", [(lambda e, wh_=wh_, c0=c0, n=n, o=o: e.dma_start(out=wh_[:, :, o:o + n], in_=w_in_v[:, :, c0:c0 + n]))
                                             for (c0, n, o) in [(1024 + h * 128, 128, 0), (1536 + h * 128, 128, 128),
                                                                (3072 + h * 256, 256, 256), (2048 + h * 256, 256, 512)]], writes=[wh_])
                            return wh_
                        wh_next = load_head(0)
                        for h in range(4):
                            wh = wh_next
                            if h + 1 < 4:
                                wh_next = load_head(h + 1)
                            s_prev = None
                            for tt in range(4):
                                tsl = slice(tt * 512, (tt + 1) * 512)
                                ur = uT_reads(tt)
                                bz = pb.get()
                                mm(bz, bz[:], [(wau_sb[:, h * 128:(h + 1) * 128], alT[:, tsl])], [wau_sb, alT])
                                a1, b1, eb, enb = tA.get(), tB.get(), ebt.get(), enbt.get()
                                l1 = a1
                                S.do("scalar", lambda e, bz=bz, a1=a1, h=h: e.activation(out=a1[:], in_=bz[:], func=AF.Exp,
                                                                                         bias=nba[:, h:h + 1], scale=-1.0),
                                     reads=[bz, nba], writes=[a1])
                                S.do("scalar", lambda e, a1=a1: e.activation(out=a1[:], in_=a1[:], func=AF.Ln, bias=1.0),
                                     writes=[a1])
                                S.do("vector", lambda e, l1=l1, b1=b1: e.tensor_tensor_scan(out=b1[:], data0=mask01[:], data1=l1[:],
                                                                                            initial=0.0, op0=ALU.mult, op1=ALU.add),
                                     reads=[l1, mask01], writes=[b1])
                                S.do("scalar", lambda e, b1=b1, eb=eb: e.activation(out=eb[:], in_=b1[:], func=AF.Exp, scale=-1.0 / 16.0),
                                     reads=[b1], writes=[eb])
                                S.do("scalar", lambda e, b1=b1, enb=enb: e.activation(out=enb[:], in_=b1[:], func=AF.Exp, scale=1.0 / 16.0),
                                     reads=[b1], writes=[enb])
                                if stop == "C2a":
                                    S.dead = True
                                bq = pb.get()
                                mm(bq, bq[:], [(wh[:, k, 0:128], uT[:, k, tsl]) for k in range(8)], [wh] + ur)
                                qs = qsT.get()
                                S.do("vector", lambda e, bq=bq, eb=eb, qs=qs: e.scalar_tensor_tensor(
                                    out=qs[:], in0=bq[:], scalar=128.0 ** -0.5, in1=eb[:], op0=ALU.mult, op1=ALU.mult),
                                    reads=[bq, eb], writes=[qs])
                                bk = pb.get()
                                mm(bk, bk[:], [(wh[:, k, 128:256], uT[:, k, tsl]) for k in range(8)], [wh] + ur)
                                ks = ksT.get()
                                S.do("vector", lambda e, bk=bk, enb=enb, ks=ks: e.tensor_tensor(out=ks[:], in0=bk[:], in1=enb[:], op=ALU.mult),
                                     reads=[bk, enb], writes=[ks])
                                vs = v_sb.get()
                                for half in range(2):
                                    bv = pb.get()

                                    def vfn(e, bv=bv, half=half, tt=tt, wh=wh):
                                        ins = None
                                        for jj in range(2):
                                            j = half * 2 + jj
                                            t0 = (tt * 4 + j) * 128
                                            for k in range(8):
                                                ins = e.matmul(bv[:, jj * 256:(jj + 1) * 256], lhsT=uT[:, k, t0:t0 + 128],
                                                               rhs=wh[:, k, 512:768], start=(k == 0), stop=(k == 7))
                                        return ins
                                    S.do("tensor", vfn, reads=[wh] + ur, writes=[bv])
                                    S.do("scalar", lambda e, bv=bv, vs=vs, half=half: e.copy(
                                        out=vs[:, half * 2:half * 2 + 2, :], in_=bv[:].rearrange("p (j n) -> p j n", j=2)),
                                        reads=[bv], writes=[vs])
                                if stop == "C2b":
                                    S.dead = True

                                def ktf(e, ks=ks):
                                    ins = None
                                    for j in range(4):
                                        ins = e.transpose(out=ptb[:, j, :], in_=ks[:, j * 128:(j + 1) * 128], identity=ident_bf[:])
                                    return ins
                                S.do("tensor", ktf, reads=[ks, ident_bf], writes=[ptb])
                                kt = kt_sb.get()
                                S.do("vector", lambda e, kt=kt: e.tensor_copy(out=kt[:], in_=ptb[:]), reads=[ptb], writes=[kt])
                                bs = pb.get()

                                def scf(e, bs=bs, ks=ks, qs=qs):
                                    ins = None
                                    for j in range(4):
                                        js = slice(j * 128, (j + 1) * 128)
                                        ins = e.matmul(bs[:, js], lhsT=ks[:, js], rhs=qs[:, js], start=True, stop=True)
                                    return ins
                                S.do("tensor", scf, reads=[ks, qs], writes=[bs])
                                sT = sT_sb.get()
                                S.do("vector", lambda e, bs=bs, sT=sT: e.tensor_tensor(out=sT[:], in0=bs[:], in1=cmask4[:], op=ALU.mult),
                                     reads=[bs, cmask4], writes=[sT])
                                bd = [pb.get(), pb.get()]
                                for half in range(2):
                                    def dsf(e, bdh=bd[half], half=half, kt=kt, vs=vs):
                                        ins = None
                                        for jj in range(2):
                                            j = half * 2 + jj
                                            ins = e.matmul(bdh[:, jj * 256:(jj + 1) * 256], lhsT=kt[:, j, :], rhs=vs[:, j, :],
                                                           start=True, stop=True)
                                        return ins
                                    S.do("tensor", dsf, reads=[kt, vs], writes=[bd[half]])
                                if stop == "C2c":
                                    S.dead = True
                                bo = [pb.get(), pb.get()]
                                for j in range(4):
                                    c = tt * 4 + j
                                    js = slice(j * 128, (j + 1) * 128)
                                    for ec in range(2):
                                        def of(e, boe=bo[ec], ec=ec, j=j, js=js, c=c, vs=vs, sT=sT, qs=qs, s_prev=s_prev):
                                            ins = e.matmul(boe[:, js], lhsT=vs[:, j, ec * 128:(ec + 1) * 128], rhs=sT[:, js],
                                                           start=True, stop=(c == 0))
                                            if c > 0:
                                                ins = e.matmul(boe[:, js], lhsT=s_prev[:, ec * 128:(ec + 1) * 128], rhs=qs[:, js],
                                                               start=False, stop=True)
                                            return ins
                                        S.do("tensor", of, reads=[vs, sT, qs] + ([s_prev] if c > 0 else []), writes=[bo[ec]])
                                    bdh = bd[j // 2]
                                    dsl = slice((j % 2) * 256, (j % 2 + 1) * 256)
                                    tm = Ttmp.get()
                                    if c == 0:
                                        S.do("vector", lambda e, bdh=bdh, dsl=dsl, tm=tm: e.tensor_copy(out=tm[:], in_=bdh[:, dsl]),
                                             reads=[bdh], writes=[tm])
                                    else:
                                        S.do("vector", lambda e, bdh=bdh, dsl=dsl, tm=tm: e.tensor_tensor(out=tm[:], in0=bdh[:, dsl], in1=Sf[:], op=ALU.add),
                                             reads=[bdh, Sf], writes=[tm])
                                    col = j * 128 + 127
                                    sbn = Sb.get()
                                    S.do("scalar", lambda e, tm=tm, eb=eb, col=col: e.activation(out=Sf[:], in_=tm[:], func=AF.Identity,
                                                                                                scale=eb[:, col:col + 1]),
                                         reads=[tm, eb], writes=[Sf])
                                    S.do("scalar", lambda e, tm=tm, eb=eb, col=col, sbn=sbn: e.activation(out=sbn[:], in_=tm[:], func=AF.Identity,
                                                                                                         scale=eb[:, col:col + 1]),
                                         reads=[tm, eb], writes=[sbn])
                                    s_prev = sbn
                                if stop == "C2d":
                                    S.dead = True
                                ot, sqq = oT_sb.get(), sq.get()
                                for ec in range(2):
                                    S.do("vector", lambda e, ot=ot, ec=ec, boe=bo[ec]: e.tensor_copy(out=ot[:, ec, :], in_=boe[:]),
                                         reads=[bo[ec]], writes=[ot])
                                    S.do("gpsimd", lambda e, sqq=sqq, ec=ec, ot=ot: e.tensor_tensor(out=sqq[:, ec, :], in0=ot[:, ec, :], in1=ot[:, ec, :], op=ALU.mult),
                                         reads=[ot], writes=[sqq])
                                bss = pb.get()
                                mm(bss, bss[:], [(ones_bf[:], sqq[:, ec, :]) for ec in range(2)], [ones_bf, sqq])
                                if stop == "C2e0":
                                    S.dead = True
                                rs = rstd.get()
                                S.do("vector", lambda e, bss=bss, rs=rs: e.tensor_scalar(out=rs[:], in0=bss[:], scalar1=1.0 / 256.0, scalar2=1e-6,
                                                                                         op0=ALU.mult, op1=ALU.add),
                                     reads=[bss], writes=[rs])
                                S.do("scalar", lambda e, rs=rs: e.activation(out=rs[:], in_=rs[:], func=AF.Sqrt), writes=[rs])
                                S.do("vector", lambda e, rs=rs: e.reciprocal(out=rs[:], in_=rs[:]), writes=[rs])
                                if stop == "C2e1":
                                    S.dead = True
                                for ec in range(2):
                                    brr = pb.get()
                                    mm(brr, brr[:], [(wh[:, k, 256 + ec * 128:256 + (ec + 1) * 128], uT[:, k, tsl]) for k in range(8)],
                                       [wh] + ur)
                                    sr = srt.get()
                                    S.do("scalar", lambda e, brr=brr, sr=sr: e.activation(out=sr[:], in_=brr[:], func=AF.Silu),
                                         reads=[brr], writes=[sr])
                                    if stop == "C2e2":
                                        S.dead = True
                                    z1 = zb.get()
                                    S.do("vector", lambda e, ot=ot, ec=ec, rs=rs, z1=z1: e.scalar_tensor_tensor(
                                        out=z1[:], in0=ot[:, ec, :], scalar=gain_sb[:, ec:ec + 1], in1=rs[:], op0=ALU.mult, op1=ALU.mult),
                                        reads=[ot, gain_sb, rs], writes=[z1])
                                    S.do("gpsimd", lambda e, z1=z1, sr=sr, h=h, ec=ec, tsl=tsl: e.tensor_tensor(
                                        out=obT[:, 2 * h + ec, tsl], in0=z1[:], in1=sr[:], op=ALU.mult),
                                        reads=[z1, sr], writes=[obT_tok[2 * h + ec][tt]])
                                if stop == "C2e":
                                    S.dead = True
                            if stop == "C2f":
                                S.dead = True
                        S.barrier()
                    dbg_dump("d_obT", obT, obT[:].rearrange("p k t -> p (k t)"), BF16)
                    if stop == "C2":
                        S.dead = True

                    with ExitStack() as ph34:
                        ya1T = sb(ph34, "ya1T", [128, 8, T], BF16)
                        with ExitStack() as ph:
                            wblk = Ring([sb(ph, "wblk%d" % i, [128, 8, 256], BF16) for i in range(2)])
                            wpg_sb = Ring([sb(ph, "wpg_sb%d" % i, [128, 2, 256], BF16) for i in range(2)])
                            psc_sb = sb(ph, "psc_sb", [128, 8], F32)
                            aT = Ring([sb(ph, "aT%d" % i, [128, T], F32) for i in range(2)])
                            sA = sb(ph, "sA", [128, T], F32)
                            sB = sb(ph, "sB", [128, T], F32)
                            pooled = Ring([sb(ph, "pooled%d" % i, [128, 2, T], BF16) for i in range(2)])
                            t15 = sb(ph, "t15", [128, 16], F32)
                            S.dma("sync", lambda e: e.dma_start(out=psc_sb[:], in_=pscT), writes=[psc_sb])
                            def load_grp(g):
                                wb_, wp_ = wblk.get(), wpg_sb.get()
                                S.dma("gpsimd", lambda e, wb_=wb_, g=g: e.dma_start(out=wb_[:], in_=w_in_v[:, :, g * 256:(g + 1) * 256]), writes=[wb_])
                                S.dma("gpsimd", lambda e, wp_=wp_, g=g: e.dma_start(out=wp_[:], in_=wpg[g].rearrange("(k p) n -> p k n", p=128)), writes=[wp_])
                                return wb_, wp_
                            grp_next = load_grp(0)
                            for g in range(4):
                                wb, wp = grp_next
                                pl = pooled.get()
                                if g + 1 < 4:
                                    grp_next = load_grp(g + 1)
                                w = 2 ** (g + 1)
                                for cc in range(2):
                                    a = aT.get()
                                    for tt in range(4):
                                        tsl = slice(tt * 512, (tt + 1) * 512)
                                        pt = pb.get()
                                        mm(pt, pt[:], [(wb[:, k, cc * 128:(cc + 1) * 128], uT[:, k, tsl]) for k in range(8)], [wb] + uT_reads(tt))
                                        S.do("scalar", lambda e, pt=pt, a=a, tsl=tsl: e.copy(out=a[:, tsl], in_=pt[:]), reads=[pt], writes=[a])
                                    src = a
                                    for stp in range(g + 1):
                                        sh = 2 ** stp
                                        dst = sA if (stp % 2 == 0) else sB
                                        S.do("vector", lambda e, src=src, dst=dst, sh=sh: e.tensor_tensor(out=dst[:, sh:], in0=src[:, sh:], in1=src[:, :T - sh], op=ALU.add),
                                             reads=[src], writes=[dst])
                                        S.do("gpsimd", lambda e, src=src, dst=dst, sh=sh: e.tensor_copy(out=dst[:, 0:sh], in_=src[:, 0:sh]),
                                             reads=[src], writes=[dst])
                                        src = dst
                                    S.do("vector", lambda e, src=src, a=a, pl=pl, cc=cc, w=w: e.scalar_tensor_tensor(
                                        out=pl[:, cc, :], in0=src[:], scalar=1.0 / w, in1=a[:], op0=ALU.mult, op1=ALU.subtract),
                                        reads=[src, a], writes=[pl])
                                    S.do("vector", lambda e, src=src, w=w: e.tensor_tensor(out=t15[:, 0:w - 1], in0=src[:, 0:w - 1], in1=rc16[:, 0:w - 1], op=ALU.mult),
                                         reads=[src, rc16], writes=[t15])
                                    S.do("vector", lambda e, a=a, pl=pl, cc=cc, w=w: e.tensor_tensor(out=pl[:, cc, 0:w - 1], in0=t15[:, 0:w - 1], in1=a[:, 0:w - 1], op=ALU.subtract),
                                         reads=[t15, a], writes=[pl])
                                for mo in range(2):
                                    for tt in range(4):
                                        tsl = slice(tt * 512, (tt + 1) * 512)
                                        pt = pb.get()
                                        mm(pt, pt[:], [(wp[:, kc, mo * 128:(mo + 1) * 128], pl[:, kc, tsl]) for kc in range(2)], [wp, pl])
                                        S.do("vector", lambda e, pt=pt, g=g, mo=mo, tsl=tsl: e.tensor_scalar(
                                            out=ya1T[:, 2 * g + mo, tsl], in0=pt[:], scalar1=psc_sb[:, 2 * g + mo:2 * g + mo + 1], scalar2=None, op0=ALU.mult),
                                            reads=[pt, psc_sb], writes=[ya1T_tok[2 * g + mo]])
                            S.barrier()
                        dbg_dump("d_ya1T", ya1T, ya1T[:].rearrange("p k t -> p (k t)"), BF16)
                        if stop == "C3":
                            S.dead = True


                        with ExitStack() as ph:
                            wg4 = Ring([sb(ph, "wg4_%d" % i, [128, 4, 8, 256], BF16) for i in range(2)])
                            sga = Ring([sb(ph, "sga%d" % i, [128, 512], F32) for i in range(2)])
                            sgb = Ring([sb(ph, "sgb%d" % i, [128, 512], F32) for i in range(2)])
                            m1 = Ring([sb(ph, "m1_%d" % i, [128, 512], F32) for i in range(2)])
                            m2 = Ring([sb(ph, "m2_%d" % i, [128, 512], F32) for i in range(2)])
                            stg = sb(ph, "stg", [128, 8, 512], BF16)
                            for tt in range(4):
                                tsl = slice(tt * 512, (tt + 1) * 512)
                                ur = uT_reads(tt)
                                obr = [obT_tok[k][tt] for k in range(8)]
                                for mo in range(8):
                                    if mo % 2 == 0:
                                        wg = wg4.get()
                                        q0 = (mo // 2) * 256
                                        srcs = [w_in_v[:, :, 4112 + q0:4112 + q0 + 256], w_in_v[:, :, 5136 + q0:5136 + q0 + 256],
                                                wba_v[:, :, q0:q0 + 256], wbb_v[:, :, q0:q0 + 256]]
                                        S.dma("gpsimd", [(lambda e, wg=wg, i4=i4, srcs=srcs: e.dma_start(out=wg[:, i4, :, :], in_=srcs[i4])) for i4 in range(4)],
                                              writes=[wg])
                                    mq = slice((mo % 2) * 128, (mo % 2 + 1) * 128)
                                    bga = pb.get()
                                    mm(bga, bga[:], [(wg[:, 0, k, mq], uT[:, k, tsl]) for k in range(8)], [wg] + ur)
                                    ga = sga.get()
                                    S.do("scalar", lambda e, bga=bga, ga=ga: e.activation(out=ga[:], in_=bga[:], func=AF.Sigmoid), reads=[bga], writes=[ga])
                                    bgb = pb.get()
                                    mm(bgb, bgb[:], [(wg[:, 1, k, mq], uT[:, k, tsl]) for k in range(8)], [wg] + ur)
                                    gb = sgb.get()
                                    S.do("scalar", lambda e, bgb=bgb, gb=gb: e.activation(out=gb[:], in_=bgb[:], func=AF.Sigmoid), reads=[bgb], writes=[gb])
                                    bya = pb.get()
                                    mm(bya, bya[:], [(wg[:, 2, k, mq], ya1T[:, k, tsl]) for k in range(8)], [wg] + ya1T_tok)
                                    mm1 = m1.get()
                                    S.do("vector", lambda e, bya=bya, ga=ga, mm1=mm1: e.tensor_tensor(out=mm1[:], in0=bya[:], in1=ga[:], op=ALU.mult),
                                         reads=[bya, ga], writes=[mm1])
                                    byb = pb.get()
                                    mm(byb, byb[:], [(wg[:, 3, k, mq], obT[:, k, tsl]) for k in range(8)], [wg] + obr)
                                    mm2 = m2.get()
                                    S.do("vector", lambda e, byb=byb, gb=gb, mm2=mm2: e.tensor_tensor(out=mm2[:], in0=byb[:], in1=gb[:], op=ALU.mult),
                                         reads=[byb, gb], writes=[mm2])
                                    S.do("vector", lambda e, mm1=mm1, mm2=mm2, mo=mo: e.tensor_tensor(out=stg[:, mo, :], in0=mm1[:], in1=mm2[:], op=ALU.add),
                                         reads=[mm1, mm2], writes=[stg])
                                S.do("scalar", lambda e, tsl=tsl: e.copy(out=obT[:, :, tsl], in_=stg[:]), reads=[stg], writes=obr)
                            S.barrier()
                    mTb = obT
                    mTb_tok = [Tok() for _ in range(NT // 4)]
                    dbg_dump("d_mT", mTb, mTb[:].rearrange("p k t -> p (k t)"), BF16)
                    if stop == "C4":
                        S.dead = True

                    with ExitStack() as phD:
                        wout_sb = sb(phD, "wout_sb", [128, 8, D], BF16)
                        g1_bc = sb(phD, "g1_bc", [128, D], F32)
                        b1_bc = sb(phD, "b1_bc", [128, D], F32)
                        wr_sb = sb(phD, "wr_sb", [128, 8, NE], F32)
                        br_bc = sb(phD, "br_bc", [128, NE], F32)
                        xin = Ring([sb(phD, "xinD%d" % i, [128, D], F32) for i in range(2)])
                        t1 = Ring([sb(phD, "t1_%d" % i, [128, D], F32) for i in range(2)])
                        zt = Ring([sb(phD, "zt%d" % i, [128, D], F32) for i in range(2)])
                        x1t = Ring([sb(phD, "x1t%d" % i, [128, D], F32) for i in range(2)])
                        u2t = Ring([sb(phD, "u2t%d" % i, [128, D], F32) for i in range(4)])
                        u2Tf = Ring([sb(phD, "u2Tf%d" % i, [128, 8, 128], F32) for i in range(4)])
                        stats = Ring([sb(phD, "stats%d" % i, [128, 2, 6], F32) for i in range(2)])
                        mv = Ring([sb(phD, "mv%d" % i, [128, 2], F32) for i in range(2)])
                        rsd = Ring([sb(phD, "rsd%d" % i, [128, 1], F32) for i in range(2)])
                        lg = Ring([sb(phD, "lg%d" % i, [128, NE], F32) for i in range(2)])
                        top8 = Ring([sb(phD, "top8_%d" % i, [128, 8], F32) for i in range(2)])
                        nmx = Ring([sb(phD, "nmx%d" % i, [128, 1], F32) for i in range(2)])
                        msk = Ring([sb(phD, "msk%d" % i, [128, NE], F32) for i in range(2)])
                        ex = Ring([sb(phD, "ex%d" % i, [128, NE], F32) for i in range(2)])
                        ssum = Ring([sb(phD, "ssum%d" % i, [128, 1], F32) for i in range(2)])
                        wtk = Ring([sb(phD, "wtk%d" % i, [128, NE], F32) for i in range(2)])
                        mkb_r = Ring([sb(phD, "mkb%d" % i, [128, NE], BF16) for i in range(2)])
                        i8_r = Ring([sb(phD, "i8_%d" % i, [128, 8], U32) for i in range(2)])
                        e4_r = Ring([sb(phD, "e4_%d" % i, [128, 4], F32) for i in range(2)])
                        ek_r = Ring([sb(phD, "ek_%d" % i, [128, 4], F32) for i in range(2)])
                        w4_r = Ring([sb(phD, "w4_%d" % i, [128, 4], F32) for i in range(2)])
                        pos_r = Ring([sb(phD, "pos_%d" % i, [128, NE], F32) for i in range(2)])
                        p4_r = Ring([sb(phD, "p4_%d" % i, [128, 4], F32) for i in range(2)])
                        oh_r = Ring([sb(phD, "oh_%d" % i, [128, NE], F32) for i in range(2)])
                        sl_r = Ring([sb(phD, "sl_%d" % i, [128, 4], F32) for i in range(2)])
                        ov_r = Ring([sb(phD, "ov_%d" % i, [128, 4], F32) for i in range(2)])
                        scf_r = Ring([sb(phD, "scf_%d" % i, [128, 4], F32) for i in range(2)])
                        sci_r = Ring([sb(phD, "sci_%d" % i, [128, 4], I32) for i in range(2)])
                        kp_r = Ring([sb(phD, "kp_%d" % i, [128, 4], F32) for i in range(2)])
                        u2b_r = Ring([sb(phD, "u2b_%d" % i, [128, D], BF16) for i in range(2)])
                        cnt_bc = sb(phD, "cnt_bc", [128, NE], F32)
                        S.do("gpsimd", lambda e: e.memset(cnt_bc[:], 0.0), writes=[cnt_bc])
                        gidx_tok, w4_tok = Tok(), Tok()
                        x1s_tok = [Tok() for _ in range(NT)]
                        for n in range(2):
                            S.dma("gpsimd", lambda e, n=n: e.dma_start(out=wout_sb[:, :, n * 512:(n + 1) * 512], in_=wout_v[:, :, n * 512:(n + 1) * 512]),
                                  writes=[wout_sb])
                        S.dma("sync", lambda e: e.dma_start(out=g1_bc[:], in_=ln1g.partition_broadcast(128)), writes=[g1_bc])
                        S.dma("sync", lambda e: e.dma_start(out=b1_bc[:], in_=ln1b.partition_broadcast(128)), writes=[b1_bc])
                        S.dma("sync", lambda e: e.dma_start(out=wr_sb[:], in_=wr_v), writes=[wr_sb])
                        S.dma("sync", lambda e: e.dma_start(out=br_bc[:], in_=br.partition_broadcast(128)), writes=[br_bc])

                        def layer_norm(zz, outt, gbc, bbc, st_, mv_, rs_, eng_gb="gpsimd", act_norm=False):
                            for c2 in range(2):
                                S.do("vector", lambda e, c2=c2: e.bn_stats(out=st_[:, c2, :], in_=zz[:, c2 * 512:(c2 + 1) * 512]),
                                     reads=[zz], writes=[st_])
                            S.do("vector", lambda e: e.bn_aggr(out=mv_[:], in_=st_[:].rearrange("p a b -> p (a b)")), reads=[st_], writes=[mv_])
                            S.do("vector", lambda e: e.tensor_scalar_add(out=rs_[:], in0=mv_[:, 1:2], scalar1=1e-5), reads=[mv_], writes=[rs_])
                            S.do("scalar", lambda e: e.activation(out=rs_[:], in_=rs_[:], func=AF.Sqrt), writes=[rs_])
                            S.do("vector", lambda e: e.reciprocal(out=rs_[:], in_=rs_[:]), writes=[rs_])
                            if act_norm:
                                S.do("vector", lambda e: e.scalar_tensor_tensor(out=mv_[:, 1:2], in0=mv_[:, 0:1], scalar=-1.0, in1=rs_[:, 0:1],
                                                                                op0=ALU.mult, op1=ALU.mult), reads=[rs_], writes=[mv_])
                                S.do("scalar", lambda e: e.activation(out=zz[:], in_=zz[:], func=AF.Identity, bias=mv_[:, 1:2], scale=rs_[:, 0:1]),
                                     reads=[mv_, rs_], writes=[zz])
                            else:
                                S.do("vector", lambda e: e.tensor_scalar(out=zz[:], in0=zz[:], scalar1=mv_[:, 0:1], scalar2=rs_[:, 0:1],
                                                                         op0=ALU.subtract, op1=ALU.mult),
                                     reads=[mv_, rs_], writes=[zz])
                            S.do(eng_gb, lambda e: e.tensor_tensor(out=zz[:], in0=zz[:], in1=gbc[:], op=ALU.mult), reads=[gbc], writes=[zz])
                            S.do(eng_gb, lambda e: e.tensor_tensor(out=outt[:], in0=zz[:], in1=bbc[:], op=ALU.add), reads=[zz, bbc], writes=[outt])

                        def stage1(i):
                            isl = slice(i * 128, (i + 1) * 128)
                            by = [pb.get(), pb.get()]
                            for n in range(2):
                                mm(by[n], by[n][:], [(mTb[:, k, isl], wout_sb[:, k, n * 512:(n + 1) * 512]) for k in range(8)],
                                   [mTb_tok[i // 4], wout_sb])
                            xi, tt1, zz, x1, u2 = xin.get(), t1.get(), zt.get(), x1t.get(), u2t.get()
                            S.dma("sync", lambda e, xi=xi, isl=isl: e.dma_start(out=xi[:], in_=x[isl, :]), writes=[xi])
                            for n in range(2):
                                nsl = slice(n * 512, (n + 1) * 512)
                                S.do("vector", lambda e, n=n, nsl=nsl, tt1=tt1, byn=by[n]: e.tensor_tensor(
                                    out=tt1[:, nsl], in0=byn[:], in1=mod_bc[:, 2 * D + n * 512:2 * D + (n + 1) * 512], op=ALU.mult),
                                    reads=[by[n], mod_bc], writes=[tt1])
                            S.do("vector", lambda e, xi=xi, tt1=tt1, zz=zz: e.scalar_tensor_tensor(out=zz[:], in0=xi[:], scalar=ALPHA, in1=tt1[:],
                                                                                                  op0=ALU.mult, op1=ALU.add),
                                 reads=[xi, tt1], writes=[zz])
                            layer_norm(zz, x1, g1_bc, b1_bc, stats.get(), mv.get(), rsd.get())
                            if stop == "D1":
                                S.dead = True
                            S.dma("sync", lambda e, x1=x1, isl=isl: e.dma_start(out=x1s[isl, :], in_=x1[:]), reads=[x1], writes=[x1s_tok[i]])
                            if dbg:
                                S.dma("sync", lambda e, x1=x1, isl=isl: e.dma_start(out=dbg_out["d_x1"][isl, :], in_=x1[:]), reads=[x1], writes=[Tok()])
                            S.do("vector", lambda e, x1=x1, u2=u2: e.tensor_tensor(out=u2[:], in0=x1[:], in1=mod_bc[:, SC_F], op=ALU.mult),
                                 reads=[x1, mod_bc], writes=[u2])
                            S.do("gpsimd", lambda e, u2=u2: e.tensor_tensor(out=u2[:], in0=u2[:], in1=mod_bc[:, SH_F], op=ALU.add),
                                 reads=[mod_bc], writes=[u2])
                            if stop == "D2":
                                S.dead = True
                            bt = [pb.get(), pb.get()]
                            for n in range(2):
                                def trf(e, n=n, u2=u2, btn=bt[n]):
                                    ins = None
                                    for kk in range(4):
                                        k = n * 4 + kk
                                        ins = e.transpose(out=btn[:, kk * 128:(kk + 1) * 128], in_=u2[:, k * 128:(k + 1) * 128], identity=ident_f[:])
                                    return ins
                                S.do("tensor", trf, reads=[u2, ident_f], writes=[bt[n]])
                            if stop == "D2a":
                                S.dead = True
                            uf = u2Tf.get()
                            for n in range(2):
                                S.do("scalar", lambda e, n=n, uf=uf, btn=bt[n]: e.copy(out=uf[:, n * 4:(n + 1) * 4, :], in_=btn[:].rearrange("p (k m) -> p k m", k=4)),
                                     reads=[bt[n]], writes=[uf])
                                if stop == "D2b":
                                    S.dead = True
                            if stop == "D3":
                                S.dead = True
                            return isl, u2, uf

                        def stage2(i, isl, u2, uf):
                            bl = pb.get()
                            mm(bl, bl[:, 0:NE], [(uf[:, k, :], wr_sb[:, k, :]) for k in range(8)], [uf, wr_sb])
                            yield
                            l_, t8, nm, ss = lg.get(), top8.get(), nmx.get(), ssum.get()
                            S.do("vector", lambda e, bl=bl, l_=l_: e.tensor_tensor(out=l_[:], in0=bl[:, 0:NE], in1=br_bc[:], op=ALU.add),
                                 reads=[bl, br_bc], writes=[l_])
                            yield
                            S.do("vector", lambda e, l_=l_, t8=t8: e.max(out=t8[:], in_=l_[:]), reads=[l_], writes=[t8])
                            yield
                            S.do("vector", lambda e, t8=t8, nm=nm: e.tensor_scalar(out=nm[:], in0=t8[:, 0:1], scalar1=-1.0, scalar2=None, op0=ALU.mult),
                                 reads=[t8], writes=[nm])
                            yield
                            mkb, i8, e4, ek, w4t = mkb_r.get(), i8_r.get(), e4_r.get(), ek_r.get(), w4_r.get()
                            S.do("vector", lambda e, l_=l_, t8=t8, mkb=mkb: e.tensor_scalar(out=mkb[:], in0=l_[:], scalar1=t8[:, 3:4], scalar2=None, op0=ALU.is_ge),
                                 reads=[l_, t8], writes=[mkb])
                            yield
                            S.do("vector", lambda e, l_=l_, t8=t8, i8=i8: e.max_index(out=i8[:], in_max=t8[:], in_values=l_[:]), reads=[l_, t8], writes=[i8])
                            yield
                            S.do("vector", lambda e, i8=i8, e4=e4: e.tensor_copy(out=e4[:], in_=i8[:, 0:4]), reads=[i8], writes=[e4])
                            yield
                            S.do("scalar", lambda e, t8=t8, nm=nm, ek=ek: e.activation(out=ek[:], in_=t8[:, 0:4], func=AF.Exp, bias=nm[:, 0:1], scale=1.0),
                                 reads=[t8, nm], writes=[ek])
                            yield
                            S.do("vector", lambda e, ek=ek, ss=ss: e.reduce_sum(out=ss[:], in_=ek[:], axis=AX.X), reads=[ek], writes=[ss])
                            yield
                            S.do("vector", lambda e, ss=ss: e.reciprocal(out=ss[:], in_=ss[:]), writes=[ss])
                            yield
                            S.do("vector", lambda e, ek=ek, ss=ss, w4t=w4t: e.tensor_scalar(out=w4t[:], in0=ek[:], scalar1=ss[:, 0:1], scalar2=None, op0=ALU.mult),
                                 reads=[ek, ss], writes=[w4t])
                            yield
                            pp = pb.get()

                            def ppf(e, pp=pp, mkb=mkb):
                                e.matmul(pp[:, 0:NE], lhsT=Ltri[:], rhs=mkb[:], start=True, stop=True)
                                return e.matmul(pp[:, NE:2 * NE], lhsT=ones_bf[:], rhs=mkb[:], start=True, stop=True)
                            S.do("tensor", ppf, reads=[Ltri, ones_bf, mkb], writes=[pp])
                            yield
                            pos, p4, sl, ov, scf, sci, kp = pos_r.get(), p4_r.get(), sl_r.get(), ov_r.get(), scf_r.get(), sci_r.get(), kp_r.get()
                            S.do("vector", lambda e, pp=pp, pos=pos: e.tensor_tensor(out=pos[:], in0=pp[:, 0:NE], in1=cnt_bc[:], op=ALU.add),
                                 reads=[pp, cnt_bc], writes=[pos])
                            yield
                            S.do("vector", lambda e, pp=pp: e.tensor_tensor(out=cnt_bc[:], in0=pp[:, NE:2 * NE], in1=cnt_bc[:], op=ALU.add),
                                 reads=[pp], writes=[cnt_bc])
                            yield
                            for k4 in range(4):
                                oh = oh_r.get()
                                S.do("vector", lambda e, oh=oh, e4=e4, k4=k4: e.tensor_scalar(out=oh[:], in0=iota32[:], scalar1=e4[:, k4:k4 + 1], scalar2=None, op0=ALU.is_equal),
                                     reads=[iota32, e4], writes=[oh])
                                yield
                                S.do("vector", lambda e, oh=oh, pos=pos: e.tensor_tensor(out=oh[:], in0=oh[:], in1=pos[:], op=ALU.mult), reads=[pos], writes=[oh])
                                yield
                                S.do("vector", lambda e, oh=oh, p4=p4, k4=k4: e.reduce_sum(out=p4[:, k4:k4 + 1], in_=oh[:], axis=AX.X), reads=[oh], writes=[p4])
                                yield
                            S.do("vector", lambda e, e4=e4, i=i: e.tensor_copy(out=e4_all[:, i * 4:(i + 1) * 4], in_=e4[:]), reads=[e4], writes=[e4a_tok])
                            yield
                            S.do("vector", lambda e, p4=p4, i=i: e.tensor_copy(out=p4_all[:, i * 4:(i + 1) * 4], in_=p4[:]), reads=[p4], writes=[p4a_tok])
                            yield
                            S.do("vector", lambda e, w4t=w4t, i=i: e.tensor_copy(out=w4_all[:, i * 4:(i + 1) * 4], in_=w4t[:]), reads=[w4t], writes=[w4_tok])
                            yield
                            u2b = u2b_r.get()
                            S.do("gpsimd", lambda e, u2=u2, u2b=u2b: e.tensor_copy(out=u2b[:], in_=u2[:]), reads=[u2], writes=[u2b])
                            yield
                            S.dma("sync", lambda e, u2b=u2b, isl=isl: e.dma_start(out=u2s[isl, :], in_=u2b[:]), reads=[u2b], writes=[u2s_tok.get()])
                            yield
                        def run2(ga, gb, stagger=3):
                            live_a, live_b, n = True, True, 0
                            while live_a or live_b:
                                if live_a:
                                    try:
                                        next(ga)
                                    except StopIteration:
                                        live_a = False
                                n += 1
                                if live_b and (n > stagger or not live_a):
                                    try:
                                        next(gb)
                                    except StopIteration:
                                        live_b = False
                        st_ = {0: stage1(0), 1: stage1(1)}
                        for p2 in range(NT // 2):
                            for j in (2 * p2 + 2, 2 * p2 + 3):
                                if j < NT:
                                    st_[j] = stage1(j)
                            run2(stage2(2 * p2, *st_.pop(2 * p2)), stage2(2 * p2 + 1, *st_.pop(2 * p2 + 1)))
                        nbk = sb(phD, "nbk", [128, NE], F32)
                        padded = sb(phD, "padded", [128, NE], F32)
                        pend = sb(phD, "pend", [128, NE], F32)
                        pstart = sb(phD, "pstart", [128, NE], F32)
                        ones32 = sb(phD, "ones32", [128, NE], F32)
                        ps_all = sb(phD, "ps_all", [128, NT * 4], F32)
                        thr = sb(phD, "thr", [128, 1], F32)
                        cmpb = sb(phD, "cmpb", [128, NE], F32)
                        bef = sb(phD, "bef", [128, 1], F32)
                        offc = sb(phD, "offc", [128, 2], F32)
                        offci = sb(phD, "offci", [128, 2], I32)
                        S.do("gpsimd", lambda e: e.memset(ones32[:], 1.0), writes=[ones32])
                        S.do("gpsimd", lambda e: e.iota(thr[:], pattern=[[0, 1]], base=0, channel_multiplier=BLK, allow_small_or_imprecise_dtypes=True), writes=[thr])
                        S.do("vector", lambda e: e.tensor_scalar(out=nbk[:], in0=cnt_bc[:], scalar1=0.0, scalar2=None, op0=ALU.is_gt), reads=[cnt_bc], writes=[nbk])
                        for j in range(1, (T + BLK - 1) // BLK):
                            S.do("vector", lambda e, j=j: e.scalar_tensor_tensor(out=nbk[:], in0=cnt_bc[:], scalar=float(j * BLK), in1=nbk[:], op0=ALU.is_gt, op1=ALU.add),
                                 reads=[cnt_bc], writes=[nbk])
                        S.do("vector", lambda e: e.tensor_scalar(out=padded[:], in0=nbk[:], scalar1=float(BLK), scalar2=None, op0=ALU.mult), reads=[nbk], writes=[padded])
                        S.do("vector", lambda e: e.tensor_tensor_scan(out=pend[:], data0=ones32[:], data1=padded[:], initial=0.0, op0=ALU.mult, op1=ALU.add),
                             reads=[ones32, padded], writes=[pend])
                        S.do("vector", lambda e: e.tensor_tensor(out=pstart[:], in0=pend[:], in1=padded[:], op=ALU.subtract), reads=[pend, padded], writes=[pstart])
                        HB = 32
                        ohs = [(tl, tl.t[:, :].rearrange("p (a b) -> p a b", b=NE)) for tl in t1.tiles]
                        for hh in range(NT * 4 // HB):
                            tl, ov_ = ohs[hh % 2]
                            csl = slice(hh * HB, (hh + 1) * HB)
                            S.do("vector", lambda e, ov_=ov_, csl=csl: e.tensor_tensor(out=ov_, in0=iota32[:].unsqueeze(1).to_broadcast([128, HB, NE]),
                                                                                      in1=e4_all[:, csl].unsqueeze(2).to_broadcast([128, HB, NE]), op=ALU.is_equal),
                                 reads=[iota32, e4a_tok], writes=[tl])
                            S.do("vector", lambda e, ov_=ov_: e.tensor_tensor(out=ov_, in0=ov_, in1=pstart[:].unsqueeze(1).to_broadcast([128, HB, NE]), op=ALU.mult),
                                 reads=[pstart], writes=[tl])
                            S.do("vector", lambda e, ov_=ov_, csl=csl: e.reduce_sum(out=ps_all[:, csl], in_=ov_, axis=AX.X), reads=[tl], writes=[ps_all])
                        S.do("vector", lambda e: e.tensor_tensor(out=ps_all[:], in0=ps_all[:], in1=p4_all[:], op=ALU.add), reads=[p4a_tok], writes=[ps_all])
                        S.do("vector", lambda e: e.tensor_copy(out=gidx_all[:], in_=ps_all[:]), reads=[ps_all], writes=[gidx_tok])
                        be_bc = sb(phD, "be_bc", [128, NBLK], F32)
                        kp = sb(phD, "kp", [128, 8], F32)
                        pidc = sb(phD, "pidc", [128, 1], F32)
                        idxWf = sb(phD, "idxWf", [128, NBLK * 8], F32)
                        idxBf = sb(phD, "idxBf", [128, NBLK], F32)
                        S.do("gpsimd", lambda e: e.iota(kp[:], pattern=[[128, 8]], base=0, channel_multiplier=1, allow_small_or_imprecise_dtypes=True), writes=[kp])
                        S.do("gpsimd", lambda e: e.iota(pidc[:], pattern=[[0, 1]], base=0, channel_multiplier=1, allow_small_or_imprecise_dtypes=True), writes=[pidc])
                        thr48 = sb(phD, "thr48", [128, NBLK], F32)
                        S.do("gpsimd", lambda e: e.iota(thr48[:], pattern=[[BLK, NBLK]], base=0, channel_multiplier=0, allow_small_or_imprecise_dtypes=True), writes=[thr48])
                        for hh, (b0, nb_) in enumerate([(0, HB), (HB, NBLK - HB)]):
                            tl, ov_ = ohs[hh % 2]
                            ovv = ov_[:, 0:nb_, :]
                            S.do("vector", lambda e, ovv=ovv, b0=b0, nb_=nb_: e.tensor_tensor(out=ovv, in0=pend[:].unsqueeze(1).to_broadcast([128, nb_, NE]),
                                                                                             in1=thr48[:, b0:b0 + nb_].unsqueeze(2).to_broadcast([128, nb_, NE]), op=ALU.is_le),
                                 reads=[pend, thr48], writes=[tl])
                            S.do("vector", lambda e, ovv=ovv, b0=b0, nb_=nb_: e.reduce_sum(out=be_bc[:, b0:b0 + nb_], in_=ovv, axis=AX.X), reads=[tl], writes=[be_bc])
                        S.do("vector", lambda e: e.tensor_copy(out=idxB2[:], in_=be_bc[:]), reads=[be_bc], writes=[idx_tok])
                        S.do("vector", lambda e: e.tensor_scalar(out=idxBf[:], in0=be_bc[:], scalar1=128.0, scalar2=pidc[:, 0:1], op0=ALU.mult, op1=ALU.add),
                             reads=[be_bc, pidc], writes=[idxBf])
                        S.do("vector", lambda e: e.tensor_copy(out=idxB[:], in_=idxBf[:]), reads=[idxBf], writes=[idx_tok])
                        S.do("vector", lambda e: e.tensor_scalar(out=be_bc[:], in0=be_bc[:], scalar1=float(D), scalar2=None, op0=ALU.mult), writes=[be_bc])
                        S.do("vector", lambda e: e.tensor_tensor(out=idxWf[:].rearrange("p (b k) -> p b k", k=8), in0=kp[:].unsqueeze(1).to_broadcast([128, NBLK, 8]),
                                                                 in1=be_bc[:].unsqueeze(2).to_broadcast([128, NBLK, 8]), op=ALU.add),
                             reads=[kp, be_bc], writes=[idxWf])
                        S.do("vector", lambda e: e.tensor_copy(out=idxW[:], in_=idxWf[:]), reads=[idxWf], writes=[idx_tok])
                        for i in range(NT):
                            isl = slice(i * 128, (i + 1) * 128)
                            u2b = u2b_r.get()
                            S.dma("sync", lambda e, u2b=u2b, isl=isl: e.dma_start(out=u2b[:], in_=u2s[isl, :]), reads=u2s_tok.tiles, writes=[u2b])
                            for k4 in range(4):
                                col = i * 4 + k4
                                S.dma("gpsimd", lambda e, u2b=u2b, col=col: e.indirect_dma_start(
                                    out=xdisp, out_offset=bass.IndirectOffsetOnAxis(ap=gidx_all[:, col:col + 1], axis=0), in_=u2b[:], in_offset=None,
                                    bounds_check=bnd_reg, oob_is_err=False), reads=[u2b, gidx_tok] + xz_tok.tiles, writes=[sc_tok.get()])
                        S.barrier()
                if stop == "D":
                    S.dead = True

            with ExitStack() as phE:
                pe = Ring([ps(phE, "pe%d" % i, [128, 512], F32) for i in range(8)])
                NRB = CAP // 128
                with ExitStack() as ph:
                    w1g_sb = Ring([sb(ph, "w1g_sb%d" % i, [128, 8, D], BF16) for i in range(2)])
                    w1u_sb = Ring([sb(ph, "w1u_sb%d" % i, [128, 8, D], BF16) for i in range(2)])
                    w2_sb = Ring([sb(ph, "w2_sb%d" % i, [128, 8, D], BF16) for i in range(2)])
                    xe_tm = Ring([sb(ph, "xe_tm%d" % i, [128, NRB, D], BF16) for i in range(2)])

                    def load_xt(bi):
                        xt_ = xe_tm.get()
                        S.dma("sync", lambda e, xt_=xt_, bi=bi: e.dma_start(
                            out=xt_[:], in_=xdisp[bi * CAP:(bi + 1) * CAP, :].rearrange("(r p) d -> p r d", p=128)),
                            reads=sc_tok.tiles + xz_tok.tiles, writes=[xt_])
                        return xt_
                    xt_next = load_xt(0)
                    xeT = Ring([sb(ph, "xeT%d" % i, [128, 8, CAP], BF16) for i in range(2)])
                    aTt = Ring([sb(ph, "aTt%d" % i, [128, 8, CAP], BF16) for i in range(2)])
                    s_r = Ring([sb(ph, "s_r%d" % i, [128, CAP], F32) for i in range(3)])
                    t_r = Ring([sb(ph, "t_r%d" % i, [128, CAP], F32) for i in range(3)])
                    ysb = Ring([sb(ph, "ysb%d" % i, [128, D], F32) for i in range(2)])
                    b2bc = Ring([sb(ph, "b2bc%d" % i, [128, D], F32) for i in range(2)])
                    bgt_r = Ring([sb(ph, "bgt%d" % i, [128, 8], F32) for i in range(2)])
                    but_r = Ring([sb(ph, "but%d" % i, [128, 8], F32) for i in range(2)])
                    WAP = [[D, 128], [128 * D, 8], [1, D]]
                    for ex_ in range(NBLK):
                        wgs, wus, w2s = w1g_sb.get(), w1u_sb.get(), w2_sb.get()
                        xT, bb, bgt, but = xeT.get(), b2bc.get(), bgt_r.get(), but_r.get()
                        xt = xt_next
                        if ex_ + 1 < NBLK:
                            xt_next = load_xt(ex_ + 1)
                        def gath(dst, tab, idxap, breg_):
                            return lambda e: e.indirect_dma_start(out=dst, out_offset=None, in_=tab,
                                                                  in_offset=bass.IndirectOffsetOnAxis(ap=idxap, axis=0),
                                                                  bounds_check=breg_, oob_is_err=False)
                        S.dma("gpsimd", [gath(wgs[:, k, :], w1g2, idxW[:, ex_ * 8 + k:ex_ * 8 + k + 1], bw_reg) for k in range(8)],
                              reads=[idx_tok], writes=[wgs])
                        S.dma("gpsimd", gath(bgt[:], b1gE2, idxB[:, ex_:ex_ + 1], bb_reg), reads=[idx_tok], writes=[bgt])
                        S.dma("gpsimd", gath(but[:], b1uE2, idxB[:, ex_:ex_ + 1], bb_reg), reads=[idx_tok], writes=[but])
                        S.dma("gpsimd", [gath(wus[:, k, :], w1u2, idxW[:, ex_ * 8 + k:ex_ * 8 + k + 1], bw_reg) for k in range(8)],
                              reads=[idx_tok], writes=[wus])
                        S.dma("gpsimd", [gath(w2s[:, k, :], w22, idxW[:, ex_ * 8 + k:ex_ * 8 + k + 1], bw_reg) for k in range(8)],
                              reads=[idx_tok], writes=[w2s])
                        S.dma("gpsimd", gath(bb[:], b2v, idxB2[:, ex_:ex_ + 1], b2_reg), reads=[idx_tok], writes=[bb])
                        S.do("vector", lambda e, bgt=bgt: e.tensor_scalar(out=bgt[:], in0=bgt[:], scalar1=1.702, scalar2=None, op0=ALU.mult), writes=[bgt])
                        S.do("vector", lambda e, but=but: e.tensor_scalar_add(out=but[:], in0=but[:], scalar1=1.0), writes=[but])
                        for rb in range(NRB):
                            pt = pe.get()
                            ptv = pt.t.bitcast(BF16)

                            def trf(e, ptv=ptv, xt=xt, rb=rb):
                                ins = None
                                for k in range(8):
                                    ins = e.transpose(out=ptv[:, k * 128:(k + 1) * 128], in_=xt[:, rb, k * 128:(k + 1) * 128], identity=ident_bf[:])
                                return ins
                            S.do("tensor", trf, reads=[xt, ident_bf], writes=[pt])
                            S.do("scalar", lambda e, ptv=ptv, xT=xT, rb=rb: e.copy(out=xT[:, :, rb * 128:(rb + 1) * 128],
                                                                              in_=ptv[:, :].rearrange("p (k m) -> p k m", k=8)),
                                 reads=[pt], writes=[xT])
                        at = aTt.get()
                        for fc in range(8):
                            bg = pe.get()
                            mm(bg, bg[:, 0:CAP], [(wgs[:, k, fc * 128:(fc + 1) * 128], xT[:, k, :]) for k in range(8)], [wgs, xT])
                            bu = pe.get()
                            mm(bu, bu[:, 0:CAP], [(wus[:, k, fc * 128:(fc + 1) * 128], xT[:, k, :]) for k in range(8)], [wus, xT])
                            s_, t_ = s_r.get(), t_r.get()
                            S.do("scalar", lambda e, bg=bg, s_=s_, fc=fc, bgt=bgt: e.activation(out=s_[:], in_=bg[:, 0:CAP], func=AF.Silu,
                                                                                           bias=bgt[:, fc:fc + 1], scale=1.702),
                                 reads=[bg, bgt], writes=[s_])
                            S.do("scalar", lambda e, bu=bu, t_=t_, fc=fc, but=but: e.activation(out=t_[:], in_=bu[:, 0:CAP], func=AF.Identity,
                                                                                           bias=but[:, fc:fc + 1], scale=1.0),
                                 reads=[bu, but], writes=[t_])
                            S.do("vector", lambda e, t_=t_: e.tensor_scalar(out=t_[:], in0=t_[:], scalar1=-6.0, scalar2=8.0, op0=ALU.max, op1=ALU.min),
                                 writes=[t_])
                            S.do("vector", lambda e, s_=s_, t_=t_, at=at, fc=fc: e.scalar_tensor_tensor(out=at[:, fc, :], in0=s_[:], scalar=C7, in1=t_[:],
                                                                                                      op0=ALU.min, op1=ALU.mult),
                                 reads=[s_, t_], writes=[at])
                        for rb in range(NRB):
                            ys = ysb.get()
                            for n in range(2):
                                nsl = slice(n * 512, (n + 1) * 512)
                                by = pe.get()
                                mm(by, by[:], [(at[:, fc, rb * 128:(rb + 1) * 128], w2s[:, fc, nsl]) for fc in range(8)], [w2s, at])
                                S.do("vector", lambda e, by=by, ys=ys, bb=bb, nsl=nsl: e.scalar_tensor_tensor(
                                    out=ys[:, nsl], in0=by[:], scalar=1.0 / 1.702, in1=bb[:, nsl], op0=ALU.mult, op1=ALU.add),
                                    reads=[by, bb], writes=[ys])
                            r0 = ex_ * CAP + rb * 128
                            S.dma("sync", lambda e, ys=ys, r0=r0: e.dma_start(out=ydisp[r0:r0 + 128, :], in_=ys[:]), reads=[ys], writes=[yw_tok.get()])
                    S.barrier()
                with ExitStack() as ph:
                    g2_bc = sb(ph, "g2_bc", [128, D], F32)
                    b2_bc = sb(ph, "b2_bc", [128, D], F32)
                    x1r = Ring([sb(ph, "x1r%d" % i, [128, D], F32) for i in range(3)])
                    yg = Ring([sb(ph, "yg%d" % i, [128, D], F32) for i in range(12)])
                    ysum = Ring([sb(ph, "ysum%d" % i, [128, D], F32) for i in range(2)])
                    zt = Ring([sb(ph, "ztE%d" % i, [128, D], F32) for i in range(2)])
                    ot_ = Ring([sb(ph, "otE%d" % i, [128, D], F32) for i in range(2)])
                    stats = Ring([sb(ph, "statsE%d" % i, [128, 2, 6], F32) for i in range(2)])
                    mv = Ring([sb(ph, "mvE%d" % i, [128, 2], F32) for i in range(2)])
                    rsd = Ring([sb(ph, "rsdE%d" % i, [128, 1], F32) for i in range(2)])
                    out_tok = Ring([Tok() for _ in range(4)])
                    S.dma("sync", lambda e: e.dma_start(out=g2_bc[:], in_=ln2g.partition_broadcast(128)), writes=[g2_bc])
                    S.dma("sync", lambda e: e.dma_start(out=b2_bc[:], in_=ln2b.partition_broadcast(128)), writes=[b2_bc])
                    def prefetch(i):
                        isl = slice(i * 128, (i + 1) * 128)
                        xr = x1r.get()
                        S.dma("sync", lambda e, xr=xr, isl=isl: e.dma_start(out=xr[:], in_=x1s[isl, :]), reads=[x1s_tok[i]], writes=[xr])
                        gs = []
                        for k4 in range(4):
                            g_ = yg.get()
                            col = i * 4 + k4
                            S.dma("gpsimd", lambda e, g_=g_, col=col: e.indirect_dma_start(
                                out=g_[:], out_offset=None, in_=ydisp, in_offset=bass.IndirectOffsetOnAxis(ap=gidx_all[:, col:col + 1], axis=0),
                                bounds_check=bnd_reg, oob_is_err=False), reads=[gidx_tok] + yw_tok.tiles, writes=[g_])
                            gs.append(g_)
                        return xr, gs
                    pre = [prefetch(0), prefetch(1)]
                    for i in range(NT):
                        isl = slice(i * 128, (i + 1) * 128)
                        if i + 2 < NT:
                            pre.append(prefetch(i + 2))
                        xr, gs = pre[i]
                        ysm, zz, oo = ysum.get(), zt.get(), ot_.get()
                        for k4 in range(4):
                            g_ = gs[k4]
                            col = i * 4 + k4
                            if k4 == 0:
                                S.do("scalar", lambda e, g_=g_, ysm=ysm, col=col: e.activation(out=ysm[:], in_=g_[:], func=AF.Identity,
                                                                                            scale=w4_all[:, col:col + 1]),
                                     reads=[g_, w4_tok], writes=[ysm])
                            else:
                                S.do("vector", lambda e, g_=g_, ysm=ysm, col=col: e.scalar_tensor_tensor(out=ysm[:], in0=g_[:], scalar=w4_all[:, col:col + 1],
                                                                                                     in1=ysm[:], op0=ALU.mult, op1=ALU.add),
                                     reads=[g_, w4_tok], writes=[ysm])
                        S.do("vector", lambda e, ysm=ysm: e.tensor_tensor(out=ysm[:], in0=ysm[:], in1=gf_bc[:], op=ALU.mult), reads=[gf_bc], writes=[ysm])
                        S.do("vector", lambda e, xr=xr, ysm=ysm, zz=zz: e.scalar_tensor_tensor(out=zz[:], in0=xr[:], scalar=ALPHA, in1=ysm[:],
                                                                                              op0=ALU.mult, op1=ALU.add),
                             reads=[xr, ysm], writes=[zz])
                        layer_norm(zz, oo, g2_bc, b2_bc, stats.get(), mv.get(), rsd.get(), eng_gb="vector", act_norm=True)
                        S.dma("sync", lambda e, oo=oo, isl=isl: e.dma_start(out=out[isl, :], in_=oo[:]), reads=[oo], writes=[out_tok.get()])
                    S.barrier()
        except _Stop:
            pass
        S.barrier(force=True)
        with nc.Block() as block:
            S.emit(block)
    return nc


_NC_CACHE = {}


def _prep_inputs(inp, b):
    f = lambda a: np.ascontiguousarray(np.asarray(a, dtype=np.float32))
    col = lambda v: f(np.asarray(v).reshape(-1, 128).T)
    wgu = np.asarray(inp["w_gate_up"])[0]
    bgu = np.asarray(inp["b_gate_up"])[0]
    m = {
        "x": f(inp["x"][b]),
        "cT": col(inp["c"][b]),
        "w_ada": f(inp["w_ada"][0]),
        "b_ada": f(inp["b_ada"][0]).reshape(1, -1),
        "w_in": f(inp["w_in"][0]),
        "wpg": f(inp["w_pool_group"][0]),
        "pscT": col(inp["pool_scale"][0]),
        "wba": f(inp["w_branch_a"][0]),
        "wau": f(inp["w_alpha_up"][0]),
        "baT": col(inp["b_alpha"][0]),
        "gainT": col(inp["gla_norm_gain"][0]),
        "wbb": f(inp["w_branch_b"][0]),
        "wout": f(inp["w_out"][0]),
        "ln1g": f(inp["ln1_gain"][0]).reshape(1, -1),
        "ln1b": f(inp["ln1_bias"][0]).reshape(1, -1),
        "wr": f(inp["w_router"][0]),
        "br": f(inp["b_router"][0]).reshape(1, -1),
        "w1g": f(wgu[:, :, 0::2]),
        "w1u": f(wgu[:, :, 1::2]),
        "b1gE": f(bgu[:, 0::2].reshape(NE, 8, 128).transpose(0, 2, 1)),
        "b1uE": f(bgu[:, 1::2].reshape(NE, 8, 128).transpose(0, 2, 1)),
        "w2": f(inp["w_down"][0]),
        "b2": f(inp["b_down"][0]),
        "ln2g": f(inp["ln2_gain"][0]).reshape(1, -1),
        "ln2b": f(inp["ln2_bias"][0]).reshape(1, -1),
    }
    return m


def kernel(**inputs):
    inputs = {k: np.asarray(v) for k, v in inputs.items()}
    if "nc" not in _NC_CACHE:
        _NC_CACHE["nc"] = build()
    nc = _NC_CACHE["nc"]
    shared = _prep_inputs(inputs, 0)
    in_maps = []
    for b in range(8):
        m = dict(shared)
        m["x"] = np.ascontiguousarray(inputs["x"][b], dtype=np.float32)
        m["cT"] = np.ascontiguousarray(np.asarray(inputs["c"][b], dtype=np.float32).reshape(-1, 128).T)
        in_maps.append(m)
    res = run_bass_kernel_spmd(nc, in_maps, core_ids=list(range(8)))
    return np.stack([np.asarray(r["out"], dtype=np.float32) for r in res.results], axis=0)
```
